# Optimizing a Trainium2 kernel written in Bass

```python
import math
import jax, jax.numpy as jnp
from jax import lax
import numpy as np

D_MODEL = 1024
BATCH = 16
SEQ = 2048
DEPTH = 2

HEAD_DIM = 64
A_HEADS = 6
A_KV_HEADS = 2
B_HEADS = 6
C_HEADS = 4
C_KV_HEADS = 2
MIX_WIDTH = HEAD_DIM * (A_HEADS + B_HEADS + C_HEADS)
PROJ_WIDTHS = (A_HEADS * HEAD_DIM, A_KV_HEADS * HEAD_DIM, A_KV_HEADS * HEAD_DIM,
               B_HEADS * HEAD_DIM, B_HEADS * HEAD_DIM, B_HEADS * HEAD_DIM,
               C_HEADS * HEAD_DIM, C_KV_HEADS * HEAD_DIM, C_KV_HEADS * HEAD_DIM)
PROJ_WIDTH = sum(PROJ_WIDTHS)
ROPE_THETA = 10000.0
GRID_W = 64
Q_BLOCK = 128
DILATED_PATTERNS = ((128, 1), (512, 4), (2048, 16))
LOCAL_HALF_WINDOW = 128
LOCAL_BLOCK = 128
PEER_HEADS = 8
N_KEYS = 128
N_EXPERTS = N_KEYS * N_KEYS
PEER_TOPK = 16
PEER_DK = 128
PEER_CHUNK = 128
NORM_EPS = 1e-6
NEG_INF = -1e30
ATTN_SCALE = HEAD_DIM ** -0.5

kernel_name = "hybrid_parallel_heads_peer_encoder"


def _rms(x):
    xf = x.astype(jnp.float32)
    y = xf * lax.rsqrt(jnp.mean(xf * xf, axis=-1, keepdims=True) + NORM_EPS)
    return y.astype(x.dtype)


def rms_norm(x, g):
    return _rms(x) * g.astype(x.dtype)


def _inv_freq(dim):
    return ROPE_THETA ** (-jnp.arange(0, dim, 2, dtype=jnp.float32) / dim)


def _rope(x, ang):
    half = x.shape[-1] // 2
    c = jnp.cos(ang)[None, :, None, :].astype(x.dtype)
    s = jnp.sin(ang)[None, :, None, :].astype(x.dtype)
    x1, x2 = x[..., :half], x[..., half:]
    return jnp.concatenate([x1 * c - x2 * s, x1 * s + x2 * c], axis=-1)


def rope_1d(x):
    pos = jnp.arange(x.shape[1], dtype=jnp.float32)
    return _rope(x, pos[:, None] * _inv_freq(HEAD_DIM)[None, :])


def rope_axial(x):
    seq = x.shape[1]
    rows = seq // GRID_W
    row = jnp.repeat(jnp.arange(rows, dtype=jnp.float32), GRID_W)
    col = jnp.tile(jnp.arange(GRID_W, dtype=jnp.float32), rows)
    half = HEAD_DIM // 2
    f = _inv_freq(half)
    return jnp.concatenate([_rope(x[..., :half], row[:, None] * f[None, :]),
                            _rope(x[..., half:], col[:, None] * f[None, :])], axis=-1)


def full_gqa(q, k, v):
    bsz, seq, h, dh = q.shape
    kv = k.shape[2]
    g = h // kv
    nb = seq // Q_BLOCK
    qb = q.reshape(bsz, nb, Q_BLOCK, kv, g, dh).transpose(1, 0, 2, 3, 4, 5)

    def block(qi):
        s = jnp.einsum('bqkgd,bskd->bkgqs', qi, k).astype(jnp.float32) * ATTN_SCALE
        p = jax.nn.softmax(s, axis=-1)
        return jnp.einsum('bkgqs,bskd->bqkgd', p.astype(v.dtype), v)

    o = lax.map(block, qb)
    return o.transpose(1, 0, 2, 3, 4, 5).reshape(bsz, seq, h, dh)


def banded_attn(q, k, v, half_window, block, sink=None):
    n, length, h, dh = q.shape
    kv = k.shape[2]
    g = h // kv
    nb = -(-length // block)
    lp = nb * block
    pad = lp - length
    qp = jnp.pad(q, ((0, 0), (0, pad), (0, 0), (0, 0))).reshape(n, nb, block, kv, g, dh)

    def kv_blocks(t):
        tp = jnp.pad(t, ((0, 0), (block, pad + block), (0, 0), (0, 0))).reshape(n, nb + 2, block, kv, dh)
        return jnp.concatenate([tp[:, :-2], tp[:, 1:-1], tp[:, 2:]], axis=2)

    kb, vb = kv_blocks(k), kv_blocks(v)
    qi = jnp.arange(lp).reshape(nb, block)
    kj = (jnp.arange(nb)[:, None] - 1) * block + jnp.arange(3 * block)[None, :]
    mask = ((jnp.abs(qi[:, :, None] - kj[:, None, :]) <= half_window)
            & (kj[:, None, :] >= 0) & (kj[:, None, :] < length))
    s = jnp.einsum('nbqkgd,nbckd->nbkgqc', qp, kb).astype(jnp.float32) * ATTN_SCALE
    s = jnp.where(mask[None, :, None, None], s, NEG_INF)
    m = jnp.max(s, axis=-1)
    if sink is not None:
        sink_b = sink.astype(jnp.float32).reshape(1, 1, kv, g, 1)
        m = jnp.maximum(m, sink_b)
    p = jnp.exp(s - m[..., None])
    l = jnp.sum(p, axis=-1)
    if sink is not None:
        l = l + jnp.exp(sink_b - m)
    o = jnp.einsum('nbkgqc,nbckd->nbqkgd', p.astype(vb.dtype), vb).astype(jnp.float32)
    o = o / l.transpose(0, 1, 4, 2, 3)[..., None]
    lse = (m + jnp.log(l)).transpose(0, 1, 4, 2, 3).reshape(n, lp, h)
    o = o.reshape(n, lp, h, dh)
    return o[:, :length].astype(q.dtype), lse[:, :length]


def dilated_attn(q, k, v):
    bsz, seq, h, dh = q.shape
    outs, lses = [], []
    for window, r in DILATED_PATTERNS:
        length = seq // r
        half = window // (2 * r)

        def to_sub(t):
            return t.reshape(bsz, length, r, h, dh).transpose(0, 2, 1, 3, 4).reshape(bsz * r, length, h, dh)

        o, lse = banded_attn(to_sub(q), to_sub(k), to_sub(v), half, half)
        outs.append(o.reshape(bsz, r, length, h, dh).transpose(0, 2, 1, 3, 4).reshape(bsz, seq, h, dh))
        lses.append(lse.reshape(bsz, r, length, h).transpose(0, 2, 1, 3).reshape(bsz, seq, h))
    alpha = jax.nn.softmax(jnp.stack(lses, axis=0), axis=0)
    o = jnp.sum(alpha[..., None] * jnp.stack(outs, axis=0).astype(jnp.float32), axis=0)
    return o.astype(q.dtype)


def peer_ffn(h, wq, keys, u, v):
    bsz, seq, d = h.shape
    hc = h.reshape((bsz * seq) // PEER_CHUNK, PEER_CHUNK, d)

    def chunk(xc):
        q = (xc @ wq).reshape(PEER_CHUNK, PEER_HEADS, 2, PEER_DK // 2)
        s = jnp.einsum('chpd,hpnd->chpn', q, keys).astype(jnp.float32)
        sv, si = lax.top_k(s, PEER_TOPK)
        cand = sv[:, :, 0, :, None] + sv[:, :, 1, None, :]
        cidx = si[:, :, 0, :, None] * N_KEYS + si[:, :, 1, None, :]
        cand = cand.reshape(PEER_CHUNK, PEER_HEADS, PEER_TOPK * PEER_TOPK)
        cidx = cidx.reshape(PEER_CHUNK, PEER_HEADS, PEER_TOPK * PEER_TOPK)
        best, pos = lax.top_k(cand, PEER_TOPK)
        idx = jnp.take_along_axis(cidx, pos, axis=-1)
        gate = jax.nn.softmax(best, axis=-1)
        ue = jnp.take(u, idx, axis=0)
        act = jax.nn.gelu(jnp.einsum('cd,chkd->chk', xc, ue).astype(jnp.float32), approximate=False) * gate
        ve = jnp.take(v, idx, axis=0)
        return jnp.einsum('chk,chkd->cd', act.astype(xc.dtype), ve)

    return lax.map(chunk, hc).reshape(bsz, seq, d)


def setup_inputs(seed: int = 0) -> dict:
    key = jax.random.key(seed)
    ks = jax.random.split(key, 12)
    f32 = jnp.float32
    nrm = lambda k, shape: jax.random.normal(k, shape, dtype=f32)
    return {
        "x": nrm(ks[0], (BATCH, SEQ, D_MODEL)),
        "attn_norm": 1.0 + 0.02 * nrm(ks[1], (DEPTH, D_MODEL)),
        "w_in": nrm(ks[2], (DEPTH, D_MODEL, PROJ_WIDTH)) * D_MODEL ** -0.5,
        "qk_gain": 1.0 + 0.02 * nrm(ks[3], (DEPTH, 3, 2, HEAD_DIM)),
        "sink_logits": 0.5 * nrm(ks[4], (DEPTH, C_HEADS)),
        "out_norm": 1.0 + 0.02 * nrm(ks[5], (DEPTH, MIX_WIDTH)),
        "w_out": nrm(ks[6], (DEPTH, MIX_WIDTH, D_MODEL)) * MIX_WIDTH ** -0.5,
        "ffn_norm": 1.0 + 0.02 * nrm(ks[7], (DEPTH, D_MODEL)),
        "peer_wq": nrm(ks[8], (DEPTH, D_MODEL, PEER_HEADS * PEER_DK)) * D_MODEL ** -0.5,
        "peer_keys": nrm(ks[9], (DEPTH, PEER_HEADS, 2, N_KEYS, PEER_DK // 2)) * (PEER_DK // 2) ** -0.5,
        "peer_u": nrm(ks[10], (DEPTH, N_EXPERTS, D_MODEL)) * D_MODEL ** -0.5,
        "peer_v": nrm(ks[11], (DEPTH, N_EXPERTS, D_MODEL)) * PEER_HEADS ** -0.5,
    }


def reference(x, attn_norm, w_in, qk_gain, sink_logits, out_norm, w_out, ffn_norm,
              peer_wq, peer_keys, peer_u, peer_v):
    bsz, seq, _ = x.shape
    bounds = [int(b) for b in np.cumsum(PROJ_WIDTHS)[:-1]]

    def heads(t):
        return t.reshape(bsz, seq, -1, HEAD_DIM)

    for layer in range(DEPTH):
        h = rms_norm(x, attn_norm[layer])
        proj = jnp.einsum('bsd,dp->bsp', h, w_in[layer])
        qa, ka, va, qb, kb, vb, qc, kc, vc = jnp.split(proj, bounds, axis=-1)
        g = qk_gain[layer]
        qa = rope_axial(rms_norm(heads(qa), g[0, 0]))
        ka = rope_axial(rms_norm(heads(ka), g[0, 1]))
        oa = full_gqa(qa, ka, heads(va))
        qb = rope_1d(rms_norm(heads(qb), g[1, 0]))
        kb = rope_1d(rms_norm(heads(kb), g[1, 1]))
        ob = dilated_attn(qb, kb, heads(vb))
        qc = rope_1d(rms_norm(heads(qc), g[2, 0]))
        kc = rope_1d(rms_norm(heads(kc), g[2, 1]))
        oc, _ = banded_attn(qc, kc, heads(vc), LOCAL_HALF_WINDOW, LOCAL_BLOCK, sink_logits[layer])
        mix = jnp.concatenate([_rms(oa.reshape(bsz, seq, -1)),
                               _rms(ob.reshape(bsz, seq, -1)),
                               _rms(oc.reshape(bsz, seq, -1))], axis=-1) * out_norm[layer].astype(x.dtype)
        x = x + jnp.einsum('bsm,md->bsd', mix, w_out[layer])
        x = x + peer_ffn(rms_norm(x, ffn_norm[layer]), peer_wq[layer], peer_keys[layer],
                         peer_u[layer], peer_v[layer])
    return x
```

```python
import numpy as np
import ml_dtypes
from contextlib import ExitStack
import concourse.bass as bass
import concourse.mybir as mybir
from concourse.bass_utils import run_bass_kernel_spmd

F32 = mybir.dt.float32
BF16 = mybir.dt.bfloat16
U32 = mybir.dt.uint32
ALU = mybir.AluOpType
AF = mybir.ActivationFunctionType
AX = mybir.AxisListType
P = 128
NT = 4096
EPS = 1e-6
NEG = -1e30

QK_CHUNKS = [(0, 1), (128, 1), (256, 1), (384, 1),
             (640, 0), (768, 0), (896, 0), (1024, 0), (1152, 0), (1280, 0),
             (1792, 0), (1920, 0), (2048, 0)]
V_RANGES = [(512, 128, 0), (1408, 384, 128), (2176, 128, 512)]


class Sched:
    ENGS = ('sp', 'act', 'dve', 'pool', 'pe')

    def __init__(s, nc, ndma=72):
        s.nc = nc
        s.sems, s.cnt, s.esem, s.dpool = [], [], {}, []
        for e in s.ENGS:
            s.esem[e] = len(s.sems); s.sems.append(nc.alloc_semaphore('e_' + e)); s.cnt.append(0)
        for i in range(ndma):
            s.dpool.append(len(s.sems)); s.sems.append(nc.alloc_semaphore('d%d' % i)); s.cnt.append(0)
        s.seen = {e: {} for e in s.ENGS}
        s.new_phase()

    def new_phase(s):
        s.stream = {e: [] for e in s.ENGS}
        s.trk, s.dsem, s.dnext = {}, {}, 0

    @staticmethod
    def _names(aps):
        out = []
        for a in aps:
            if a is None or isinstance(a, (int, float)):
                continue
            if isinstance(a, str):
                out.append(a); continue
            if str(a.space) == 'DRAM':
                continue
            out.append(a.tensor.name)
        return out

    def op(s, eng, fn, reads=(), writes=(), dma_on=None, sreads=()):
        rn, wn = s._names(reads) + s._names(sreads), s._names(writes)
        own = s.esem[eng]
        raw, war = {}, {}
        for n in rn:
            t = s.trk.get(n)
            if t:
                for k, v in t[0].items():
                    raw[k] = max(raw.get(k, 0), v)
        for n in wn:
            t = s.trk.get(n)
            if t:
                for d in t:
                    for k, v in d.items():
                        war[k] = max(war.get(k, 0), v)
        deps = dict(raw)
        if eng == 'pe':
            deps.pop(own, None)
        for k, v in war.items():
            if k == own and dma_on is None:
                continue
            deps[k] = max(deps.get(k, 0), v)
        waits = []
        for k, v in deps.items():
            if s.seen[eng].get(k, 0) < v:
                waits.append((k, v)); s.seen[eng][k] = v
        if dma_on is None:
            k = own; inc = 1
        else:
            name = dma_on.tensor.name
            if name not in s.dsem:
                s.dsem[name] = s.dpool[s.dnext]; s.dnext += 1
            k = s.dsem[name]; inc = 16
        s.cnt[k] += inc
        v = s.cnt[k]
        for n in rn:
            s.trk.setdefault(n, ({}, {}))[1][k] = v
        for n in wn:
            s.trk.setdefault(n, ({}, {}))[0][k] = v
        s.stream[eng].append((waits, fn, k, inc))

    def barrier(s):
        for e in s.ENGS:
            waits = []
            for k in range(len(s.sems)):
                if s.cnt[k] > 0 and s.seen[e].get(k, 0) < s.cnt[k]:
                    waits.append((k, s.cnt[k])); s.seen[e][k] = s.cnt[k]
            s.stream[e].append((waits, None, 0, 0))

    def emit(s):
        nc = s.nc
        s.barrier()
        with nc.Block() as blk:
            def mk(name):
                def body(e):
                    for waits, fn, k, inc in s.stream[name]:
                        for (sk, v) in waits:
                            e.wait_ge(s.sems[sk], v)
                        if fn is not None:
                            fn(e).then_inc(s.sems[k], inc)
                return body
            blk.sync(mk('sp')); blk.scalar(mk('act')); blk.vector(mk('dve'))
            blk.gpsimd(mk('pool')); blk.tensor(mk('pe'))
        s.new_phase()


def build(stop_after=None, dbg=False):
    nc = bass.Bass("TRN2", target_bir_lowering=False)

    def din(name, shape, dt=F32):
        return nc.dram_tensor(name, list(shape), dt, kind="ExternalInput").ap()

    def dscr(name, shape, dt, big=False):
        kind = "ExternalOutput" if (dbg and not big) else "Internal"
        return nc.dram_tensor(name, list(shape), dt, kind=kind).ap()

    x = din("x", [NT, 1024])
    attn_norm = din("attn_norm", [2, 1024]); out_norm = din("out_norm", [2, 1024]); ffn_norm = din("ffn_norm", [2, 1024])
    w_in = din("w_in", [2, 1024, 2304]); w_out = din("w_out", [2, 1024, 1024]); wq = din("wq", [2, 1024, 1024])
    gaincol = din("gaincol", [2, P, 13]); sink = din("sink", [1, 8]); keysbd = din("keysbd", [2, P, 8, 256])
    pu = din("pu", [2, 16384, 1024]); pv = din("pv", [2, 16384, 1024])
    c_identb = din("c_identb", [P, P], BF16); c_identf = din("c_identf", [P, P])
    c_ct1 = din("c_ct1", [P, 2048]); c_st1 = din("c_st1", [P, 2048]); c_cta = din("c_cta", [P, 2048]); c_sta = din("c_sta", [P, 2048])
    c_psw1 = din("c_psw1", [P, P], BF16); c_pswa = din("c_pswa", [P, P], BF16); c_bones = din("c_bones", [P, P], BF16)
    c_tb = din("c_tb", [P, 3968], BF16); c_tc = din("c_tc", [P, 3968], BF16)
    c_iota = din("c_iota", [P, P]); c_io16 = din("c_io16", [P, 16])
    out = nc.dram_tensor("out", [NT, 1024], F32, kind="ExternalOutput").ap()

    UVb = dscr("UVb", [2, 128, P, 2048], BF16, big=True)
    QKT = dscr("QKT", [13, P, NT], BF16)
    Vtok = dscr("Vtok", [NT, 640], BF16)
    MIX = dscr("MIX", [NT, 1024], F32)
    HT = dscr("HT", [8, P, NT], BF16)
    RT = dscr("RT", [3, P, NT], F32)

    S = Sched(nc)
    uid = [0]

    def nm(n):
        uid[0] += 1
        return '%s_%d' % (n, uid[0])

    def mm(o, lhsT, rhs, start=True, stop=True, okey=None):
        S.op('pe', lambda e: e.matmul(o, lhsT, rhs, start=start, stop=stop), [lhsT, rhs], [okey or o])

    def tr(o, i, ident):
        S.op('pe', lambda e: e.transpose(o, i, ident), [i, ident], [o])

    def act(o, i, func, bias=None, scale=None, accum=None, ikey=None):
        kw = {}
        if bias is not None: kw['bias'] = bias
        if scale is not None: kw['scale'] = scale
        if accum is not None: kw['accum_out'] = accum
        S.op('act', lambda e: e.activation(o, i, func, **kw), [ikey or i], [o, accum], sreads=[bias, scale])

    def tt(eng, o, a, b, op):
        en = {'dve': 'dve', 'pool': 'pool'}[eng]
        S.op(en, lambda e: e.tensor_tensor(o, a, b, op), [a, b], [o])

    def ts(eng, o, a, s1, s2, op0, op1=None):
        if op1 is None:
            S.op(eng, lambda e: e.tensor_scalar(o, a, s1, s2, op0), [a], [o], sreads=[s1, s2])
        else:
            S.op(eng, lambda e: e.tensor_scalar(o, a, s1, s2, op0, op1), [a], [o], sreads=[s1, s2])

    def stt(o, a, sc, b, op0, op1):
        S.op('dve', lambda e: e.scalar_tensor_tensor(o, a, sc, b, op0, op1), [a, b], [o], sreads=[sc])

    def cp(eng, o, i):
        if eng == 'act':
            S.op('act', lambda e: e.copy(o, i), [i], [o])
        else:
            S.op(eng, lambda e: e.tensor_copy(o, i), [i], [o])

    def recip(o, i):
        S.op('dve', lambda e: e.reciprocal(o, i), [i], [o])

    def recip_fast(o, i):
        S.op('dve', lambda e: e.reciprocal_approx_fast(o, i), [i], [o])

    def red(o, i, op=ALU.add):
        S.op('dve', lambda e: e.tensor_reduce(o, i, AX.X, op), [i], [o])

    def memset(eng, o, val):
        S.op(eng, lambda e: e.memset(o, val), [], [o])

    def dma(o, i, eng='sp'):
        sbside = o if str(o.space) != 'DRAM' else i
        S.op(eng, lambda e: e.dma_start(out=o, in_=i), [i], [o], dma_on=sbside)

    def vmax(o, i):
        S.op('dve', lambda e: e.max(o, i), [i], [o])

    def vmaxidx(o, mx, vals):
        S.op('dve', lambda e: e.max_index(o, mx, vals), [mx, vals], [o])

    def vmrep(o, rep, vals, imm):
        S.op('dve', lambda e: e.match_replace(o, rep, vals, imm), [rep, vals], [o])

    def tss(o, i, sc, op):
        S.op('dve', lambda e: e.tensor_single_scalar(o, i, sc, op), [i], [o])

    def prepass():
        with ExitStack() as es:
            sb = lambda n, sh, dt: es.enter_context(nc.sbuf_tensor(nm(n), sh, dt))
            pst = lambda n, sh, dt: es.enter_context(nc.psum_tensor(nm(n), sh, dt))
            uf = [sb("uf%d" % i, [P, 1024], F32) for i in range(3)]
            vf = [sb("vf%d" % i, [P, 1024], F32) for i in range(3)]
            ub = [sb("ub%d" % i, [P, 1024], BF16) for i in range(2)]
            vb = [sb("vb%d" % i, [P, 1024], BF16) for i in range(2)]
            ut = [sb("ut%d" % i, [P, 1024], BF16) for i in range(2)]
            pt = [pst("pt%d" % i, [P, 1024], BF16) for i in range(2)]
            idb = sb("idb", [P, P], BF16)
            dma(idb[:], c_identb)
            blocks = [(l, c) for l in range(2) for c in range(128)]

            def load(i):
                l, c = blocks[i]
                dma(uf[i % 3][:], pu[l, c * 128:(c + 1) * 128, :])
                dma(vf[i % 3][:], pv[l, c * 128:(c + 1) * 128, :])
            load(0); load(1)
            for i, (l, c) in enumerate(blocks):
                if i + 2 < len(blocks):
                    load(i + 2)
                cp('pool', ub[i % 2][:], uf[i % 3][:])
                cp('act', vb[i % 2][:], vf[i % 3][:])
                for dc in range(8):
                    tr(pt[i % 2][:, dc * 128:(dc + 1) * 128], ub[i % 2][:, dc * 128:(dc + 1) * 128], idb[:])
                cp('dve', ut[i % 2][:], pt[i % 2][:])
                dma(UVb[l, c, :, 0:1024], ut[i % 2][:])
                dma(UVb[l, c, :, 1024:2048], vb[i % 2][:])
            S.emit()

    def phase1(l, xsrc):
        with ExitStack() as es:
            sb = lambda n, sh, dt: es.enter_context(nc.sbuf_tensor(nm(n), sh, dt))
            pst = lambda n, sh, dt: es.enter_context(nc.psum_tensor(nm(n), sh, dt))
            Wb = sb("Wb", [P, 8, 2304], BF16)
            wtmp = [sb("wtmp%d" % i, [P, 2304], F32) for i in range(2)]
            gbc = sb("gbc", [P, 1024], F32)
            ct1 = sb("ct1", [P, 2048], F32); st1 = sb("st1", [P, 2048], F32)
            cta = sb("cta", [P, 2048], F32); sta = sb("sta", [P, 2048], F32)
            gcol = sb("gcol", [P, 13], F32)
            bones = sb("bones", [P, P], BF16); psw1 = sb("psw1", [P, P], BF16); pswa = sb("pswa", [P, P], BF16)
            idb = sb("idb", [P, P], BF16)
            epsT = sb("epsT", [P, 1], F32)
            xt = [sb("xt%d" % i, [P, 4, 1024], F32) for i in range(2)]
            junk = sb("junk", [P, 1024], BF16)
            ssq = sb("ssq", [P, 4], F32); sd4 = sb("sd4", [P, 4], F32); rstd = sb("rstd", [P, 4], F32)
            hn = [sb("hn%d" % i, [P, 1024], BF16) for i in range(2)]
            hnT = [sb("hnT%d" % i, [P, 8, 512], BF16) for i in range(2)]
            vtok = [sb("vtok%d" % i, [P, 640], BF16) for i in range(2)]
            sq = [sb("sq%d" % i, [P, 512], BF16) for i in range(2)]
            sdt = [sb("sdt%d" % i, [P, 512], F32) for i in range(2)]
            rs = [sb("rs%d" % i, [P, 512], F32) for i in range(2)]
            xn = [sb("xn%d" % i, [P, 512], BF16) for i in range(2)]
            t1 = [sb("t1_%d" % i, [P, 512], F32) for i in range(2)]
            t2 = [sb("t2_%d" % i, [P, 512], F32) for i in range(2)]
            qk = [sb("qk%d" % i, [P, 512], BF16) for i in range(2)]
            tp = pst("tp", [P, 1024], BF16)
            pvA = pst("pvA", [P, 512], F32); pvB = pst("pvB", [P, 512], F32)
            pq = [pst("pq%d" % i, [P, 512], F32) for i in range(3)]
            pss = pst("pss", [P, 512], F32)
            psw = [pst("psw%d" % i, [P, 512], F32) for i in range(1)]

            def load(tb):
                dma(xt[tb % 2][:], xsrc[tb * 512:(tb + 1) * 512, :].rearrange("(t p) d -> p t d", p=P))
            load(0)
            memset('dve', epsT[:], EPS)
            dma(gbc[:], attn_norm[l:l + 1, :].to_broadcast([P, 1024]))
            dma(idb[:], c_identb)
            dma(gcol[:], gaincol[l]); dma(bones[:], c_bones); dma(psw1[:], c_psw1); dma(pswa[:], c_pswa)
            for dc in range(8):
                dma(wtmp[dc % 2][:], w_in[l, dc * 128:(dc + 1) * 128, :])
                cp('act' if dc % 2 else 'dve', Wb[:, dc, :], wtmp[dc % 2][:])
            dma(ct1[:], c_ct1); dma(st1[:], c_st1); dma(cta[:], c_cta); dma(sta[:], c_sta)

            cc = 0
            for tb in range(8):
                if tb + 1 < 8:
                    load(tb + 1)
                X = xt[tb % 2]; HTt = hnT[tb % 2]
                pos0 = (tb % 4) * 512
                for t in range(4):
                    act(junk[:], X[:, t, :], AF.Square, accum=ssq[:, t:t + 1])
                act(sd4[:], ssq[:], AF.Sqrt, bias=epsT[:, 0:1], scale=1.0 / 1024)
                recip(rstd[:], sd4[:])
                for t in range(4):
                    H = hn[t % 2]
                    stt(H[:], X[:, t, :], rstd[:, t:t + 1], gbc[:], ALU.mult, ALU.mult)
                    for dc in range(8):
                        tr(tp[:, dc * 128:(dc + 1) * 128], H[:, dc * 128:(dc + 1) * 128], idb[:])
                    cp('act', HTt[:, :, t * 128:(t + 1) * 128], tp[:].rearrange("p (c t) -> p c t", c=8))
                for t in range(4):
                    VT = vtok[t % 2]
                    for (c0, w, oc) in V_RANGES:
                        dst = pvA[:, oc:oc + w] if oc < 512 else pvB[:, 0:w]
                        for dc in range(8):
                            mm(dst, HTt[:, dc, t * 128:(t + 1) * 128], Wb[:, dc, c0:c0 + w], start=(dc == 0), stop=(dc == 7))
                    cp('act', VT[:, 0:512], pvA[:])
                    cp('dve', VT[:, 512:640], pvB[:, 0:128])
                    r0 = tb * 512 + t * 128
                    dma(Vtok[r0:r0 + 128, :], VT[:])
                def s1(ci):
                    c0, ax = QK_CHUNKS[ci]; k = ci % 2; k3 = ci % 3
                    for dc in range(8):
                        mm(pq[k3][:], Wb[:, dc, c0:c0 + 128], HTt[:, dc, :], start=(dc == 0), stop=(dc == 7))
                    act(sq[k][:], pq[k3][:], AF.Square)

                def s2(ci):
                    k = ci % 2; k3 = ci % 3
                    mm(pss[:], bones[:], sq[k][:])
                    act(sdt[k][:], pss[:], AF.Ln, bias=epsT[:, 0:1], scale=1.0 / 64)
                    act(rs[k][:], sdt[k][:], AF.Exp, scale=-0.5)
                    stt(xn[k][:], pq[k3][:], gcol[:, ci:ci + 1], rs[k][:], ALU.mult, ALU.mult)

                def s3(ci):
                    c0, ax = QK_CHUNKS[ci]; k = ci % 2
                    CT, ST, PSW = (cta, sta, pswa) if ax else (ct1, st1, psw1)
                    mm(psw[0][:], PSW[:], xn[k][:])
                    tt('pool', t1[k][:], xn[k][:], CT[:, pos0:pos0 + 512], ALU.mult)
                    tt('dve', t2[k][:], psw[0][:], ST[:, pos0:pos0 + 512], ALU.mult)
                    tt('pool', qk[k][:], t1[k][:], t2[k][:], ALU.add)
                    dma(QKT[ci, :, tb * 512:(tb + 1) * 512], qk[k][:])
                NCH = len(QK_CHUNKS)
                s1(0); s1(1); s2(0)
                for ci in range(NCH):
                    if ci + 2 < NCH:
                        s1(ci + 2)
                    if ci + 1 < NCH:
                        s2(ci + 1)
                    s3(ci)
            S.emit()

    def phase2(l):
        with ExitStack() as es:
            sb = lambda n, sh, dt: es.enter_context(nc.sbuf_tensor(nm(n), sh, dt))
            pst = lambda n, sh, dt: es.enter_context(nc.psum_tensor(nm(n), sh, dt))
            KT = [sb("KT%d" % i, [P, 2048], BF16) for i in range(2)]
            QTh = [[sb("QT%d_%d" % (h_, i), [P, 2048], BF16) for i in range(2)] for h_ in range(2)]
            Vp = [sb("Vp%d" % i, [P, 16, 65], BF16) for i in range(2)]
            tbm = sb("tbm", [P, 3968], BF16); tcm = sb("tcm", [P, 3968], BF16)
            mix = sb("mixsb", [P, 16, 1024], F32)
            NPS = 5
            pe_ = [sb("pe%d" % i, [P, 512], BF16) for i in range(NPS)]
            snk = sb("snk", [P, 8], F32); esk = sb("esk", [P, 8], F32)
            lt = [sb("lt%d" % i, [P, 1], F32) for i in range(4)]
            rl = [sb("rl%d" % i, [P, 1], F32) for i in range(4)]
            psc = [pst("psc%d" % i, [P, 512], F32) for i in range(NPS)]
            pot = [pst("pot%d" % i, [P, 512], F32) for i in range(1)]
            ptr = pst("ptr", [P, 512], F32)
            ot_sb = [sb("ot_sb%d" % i, [P, 512], F32) for i in range(2)]
            lt4 = sb("lt4", [P, 4], F32); rl4 = sb("rl4", [P, 4], F32)
            idf = sb("idf", [P, P], F32)
            dma(idf[:], c_identf)
            uf = [sb("uf%d" % i, [P, 1024], F32) for i in range(3)]
            vf = [sb("vf%d" % i, [P, 1024], F32) for i in range(3)]
            ub = [sb("ub%d" % i, [P, 1024], BF16) for i in range(2)]
            vb = [sb("vb%d" % i, [P, 1024], BF16) for i in range(2)]
            ut = [sb("ut%d" % i, [P, 1024], BF16) for i in range(2)]
            ptp = pst("ptp", [P, 1024], BF16)
            idb = sb("idb", [P, P], BF16)
            dma(idb[:], c_identb)

            def pre_load(i):
                if i < 128:
                    dma(uf[i % 3][:], pu[l, i * 128:(i + 1) * 128, :])
                    dma(vf[i % 3][:], pv[l, i * 128:(i + 1) * 128, :])

            def pre_cast(i):
                cp('pool', ub[i % 2][:], uf[i % 3][:])
                cp('pool', vb[i % 2][:], vf[i % 3][:])
                dma(UVb[l, i, :, 1024:2048], vb[i % 2][:])

            def pre_tr(i):
                if i < 0:
                    return
                for dc in range(8):
                    tr(ptp[:, dc * 128:(dc + 1) * 128], ub[i % 2][:, dc * 128:(dc + 1) * 128], idb[:])
                cp('dve', ut[i % 2][:], ptp[:])
                dma(UVb[l, i, :, 0:1024], ut[i % 2][:])
            pre_load(0); pre_load(1)

            dma(tbm[:], c_tb); dma(tcm[:], c_tc)
            dma(snk[:], sink[0:1, :].to_broadcast([P, 8]))
            m8 = sb("m8", [P, 1], F32)
            memset('dve', m8[:], -8.0)
            act(esk[:], snk[:], AF.Exp, bias=m8[:, 0:1])
            for i in range(2):
                memset('pool', Vp[i][:], 1.0)
                for h_ in range(2):
                    memset('pool', QTh[h_][i][:], 0.0)

            groups = []
            for g in range(2):
                groups.append(('A', 3, g, g * 64, [(h // 2, h % 2, h * 64, None) for h in range(3 * g, 3 * g + 3)]))
            for h in range(6):
                groups.append(('B', 7 + h // 2, h % 2, 128 + h * 64, [(4 + h // 2, h % 2, 384 + h * 64, None)]))
            for g in range(2):
                groups.append(('C', 12, g, 512 + g * 64, [(10 + h // 2, h % 2, 768 + h * 64, h) for h in range(2 * g, 2 * g + 2)]))

            units = []
            kvc = 0; qc = 0
            for b in range(2):
                for (kind, kch, kh, vcol, qheads) in groups:
                    for qi, (qch, qh, mcol, sidx) in enumerate(qheads):
                        for qb in range(4):
                            units.append(dict(b=b, kind=kind, kch=kch, kh=kh, vcol=vcol, qch=qch, qh=qh, mcol=mcol,
                                              sidx=sidx, qb=qb, kvi=kvc, qi=qc, newkv=(qi == 0 and qb == 0), newq=(qb == 0)))
                        qc += 1
                    kvc += 1
            for i_, u in enumerate(units):
                u['lastb'] = (i_ + 1 == len(units)) or (units[i_ + 1]['b'] != u['b'])
            ecs = [0]

            def qk_mm(u, i):
                kt = u['kts'][i]; K = u['K']; Q = u['Q']; qb = u['qb']; sl = u['slots'][i]
                if u['kind'] == 'A':
                    mm(psc[sl][:], K[:, kt * 128:(kt + 1) * 128], Q[:, qb * 512:(qb + 1) * 512])
                else:
                    M = tbm if u['kind'] == 'B' else tcm
                    cst = (qb * 4 - kt + 15) * 128
                    mm(psc[sl][:], K[:, kt * 128:(kt + 1) * 128], Q[:, qb * 512:(qb + 1) * 512], start=True, stop=False)
                    mm(psc[sl][:], idb[:], M[:, cst:cst + 512], start=False, stop=True)

            def start_unit(u):
                t0 = u['b'] * 2048; kh = u['kh']
                u['K'] = KT[u['kvi'] % 2]; u['V'] = Vp[u['kvi'] % 2]; u['Q'] = QTh[kh][u['qi'] % 2]
                if u['newkv']:
                    dma(u['K'][:], QKT[u['kch'], :, t0:t0 + 2048])
                    dma(u['V'][:, :, 0:64], Vtok[t0:t0 + 2048, u['vcol']:u['vcol'] + 64].rearrange("(t p) c -> p t c", p=P))
                if u['newq']:
                    dma(u['Q'][kh * 64:(kh + 1) * 64, :], QKT[u['qch'], u['qh'] * 64:(u['qh'] + 1) * 64, t0:t0 + 2048])
                qt0 = u['qb'] * 4
                rad = {'A': 99, 'B': 8, 'C': 1}[u['kind']]
                kts = [kt for kt in range(16) if any(abs(qt0 + j - kt) <= rad for j in range(4))]
                u['kts'] = kts
                u['slots'] = [(ecs[0] + i) % NPS for i in range(len(kts))]
                ecs[0] += len(kts)
                for i in range(min(NPS - 1, len(kts))):
                    qk_mm(u, i)

            start_unit(units[0])
            for unit, u in enumerate(units):
                pre_load(unit + 2)
                pre_tr(unit - 1)
                kts = u['kts']; nk = len(kts); V = u['V']; qt0 = u['qb'] * 4; mcol = u['mcol']; sidx = u['sidx']
                for i, kt in enumerate(kts):
                    if i + NPS - 1 < nk:
                        qk_mm(u, i + NPS - 1)
                    ps_ = psc[u['slots'][i]]; pe = pe_[u['slots'][i]]
                    act(pe[:], ps_[:], AF.Exp, bias=m8[:, 0:1], scale=0.125)
                    mm(pot[0][0:65, :], V[:, kt, :], pe[:], start=(i == 0), stop=(i == nk - 1))
                if unit + 1 < len(units):
                    start_unit(units[unit + 1])
                osb = ot_sb[unit % 2]
                cp('dve', osb[0:65, :], pot[0][0:65, :])
                for j in range(4):
                    tr(ptr[:, j * 65:(j + 1) * 65], osb[0:65, j * 128:(j + 1) * 128], idf[0:65, 0:65])
                ptr3 = ptr[:, 0:260].rearrange("p (j c) -> p j c", j=4)
                if sidx is not None:
                    ts('dve', lt4[:], ptr3[:, :, 64], esk[:, l * 4 + sidx:l * 4 + sidx + 1], None, ALU.add)
                    recip(rl4[:], lt4[:])
                else:
                    recip(rl4[:], ptr3[:, :, 64])
                tt('dve', mix[:, qt0:qt0 + 4, mcol:mcol + 64], ptr3[:, :, 0:64],
                   rl4[:][:, :, None].to_broadcast([P, 4, 64]), ALU.mult)
                pre_cast(unit)
                if u['lastb']:
                    t0 = u['b'] * 2048
                    dma(MIX[t0:t0 + 2048, :].rearrange("(t p) d -> p t d", p=P), mix[:])
            pre_tr(127)
            S.emit()

    def phase3(l, xsrc):
        with ExitStack() as es:
            sb = lambda n, sh, dt: es.enter_context(nc.sbuf_tensor(nm(n), sh, dt))
            pst = lambda n, sh, dt: es.enter_context(nc.psum_tensor(nm(n), sh, dt))
            Wo = sb("Wo", [P, 8, 1024], BF16); Wqb = sb("Wqb", [P, 8, 1024], BF16)
            wtmp = [sb("wtmp%d" % i, [P, 1024], F32) for i in range(4)]
            kbd = sb("kbd", [P, 8, 256], F32)
            gout = sb("gout", [P, 1024], F32); gffn = sb("gffn", [P, 1024], F32)
            idb = sb("idb", [P, P], BF16); idf = sb("idf", [P, P], F32)
            io16 = sb("io16", [P, 16], F32)
            epsT = sb("epsT", [P, 1], F32)
            mx = [sb("mx%d" % i, [P, 1024], F32) for i in range(3)]
            xx = [sb("xx%d" % i, [P, 1024], F32) for i in range(3)]
            junk = sb("junk", [P, 1024], BF16)
            s3b = [sb("s3b%d" % i, [P, 4], F32) for i in range(2)]; d3b = [sb("d3b%d" % i, [P, 4], F32) for i in range(2)]
            s3 = sb("s3", [P, 4], F32); d3 = sb("d3", [P, 4], F32); r3 = sb("r3", [P, 4], F32)
            mixn = sb("mixn", [P, 1024], BF16)
            mixT = sb("mixT", [P, 8, 128], BF16)
            xnew = [sb("xnew%d" % i, [P, 1024], F32) for i in range(2)]
            hn2 = sb("hn2", [P, 1024], BF16)
            h2T = [sb("h2T%d" % i, [P, 8, 128], BF16) for i in range(2)]
            qT = sb("qT", [P, 8, 128], F32)
            sc2 = [sb("sc%d" % i, [P, 2048], F32) for i in range(2)]
            junk2 = sb("junk2", [P, 1024], BF16)
            mtmp = sb("mtmp", [P, 1024], F32); wo_sb = sb("wo_sb", [P, 1024], F32)
            s4 = sb("s4", [P, 1], F32); d4 = sb("d4", [P, 1], F32); r4 = sb("r4", [P, 1], F32)
            wk = sb("wk", [P, 2048], F32)
            sv = sb("sv", [P, 16, 16], F32); si = sb("si", [P, 16, 16], U32); sif = sb("sif", [P, 16, 16], F32)
            cand = sb("cand", [P, 8, 256], F32); wk2 = sb("wk2", [P, 8, 256], F32)
            best = sb("best", [P, 8, 16], F32); pos = sb("pos", [P, 8, 16], U32)
            dlt = sb("dlt", [P, 8, 16], F32); ex = sb("ex", [P, 8, 16], F32)
            zz = sb("zz", [P, 8], F32); rz = sb("rz", [P, 8], F32)
            pa = sb("pa", [P, 8, 16], U32); pb = sb("pb", [P, 8, 16], U32)
            paf = sb("paf", [P, 8, 16], F32); pbf = sb("pbf", [P, 8, 16], F32)
            eq = sb("eq", [P, 8, 16, 16], F32); pr = sb("pr", [P, 8, 16, 16], F32)
            IJG = sb("IJG", [P, 3, 128], F32)
            rt = [sb("rt%d" % i, [P, 3, 128], F32) for i in range(2)]
            tp = pst("tp", [P, 1024], BF16)
            pA = pst("pA", [P, 1024], F32)
            pS = pst("pS", [P, 2048], F32)
            pR = pst("pR", [P, 512], F32)

            memset('dve', epsT[:], EPS)

            def load(ti):
                dma(mx[ti % 3][:], MIX[ti * 128:(ti + 1) * 128, :])
                dma(xx[ti % 3][:], xsrc[ti * 128:(ti + 1) * 128, :])
            GR = [(0, 384), (384, 384), (768, 256)]
            sv4 = sv[:].rearrange("p (h t) k -> p h t k", t=2)
            sif4 = sif[:].rearrange("p (h t) k -> p h t k", t=2)
            cand4 = cand[:].rearrange("p h (a b) -> p h a b", a=16)

            def A0(ti):
                M = mx[ti % 3]; S3 = s3b[ti % 2]; D3 = d3b[ti % 2]
                for g, (c0, w) in enumerate(GR):
                    act(junk[:, 0:w], M[:, c0:c0 + w], AF.Square, scale=float(w) ** -0.5, accum=S3[:, g:g + 1])
                act(D3[:, 0:3], S3[:, 0:3], AF.Sqrt, bias=epsT[:, 0:1], scale=1.0)

            def A1(ti):
                M = mx[ti % 3]; D3 = d3b[ti % 2]
                recip(r3[:, 0:3], D3[:, 0:3])
                for g, (c0, w) in enumerate(GR):
                    ts('pool', mtmp[:, c0:c0 + w], M[:, c0:c0 + w], r3[:, g:g + 1], 1.0, ALU.mult, ALU.mult)
                tt('pool', mixn[:], mtmp[:], gout[:], ALU.mult)
                for dc in range(8):
                    tr(tp[:, dc * 128:(dc + 1) * 128], mixn[:, dc * 128:(dc + 1) * 128], idb[:])
                cp('act', mixT[:], tp[:].rearrange("p (c t) -> p c t", c=8))
                for hf in range(2):
                    for dc in range(8):
                        mm(pA[:, hf * 512:(hf + 1) * 512], mixT[:, dc, :], Wo[:, dc, hf * 512:(hf + 1) * 512],
                           start=(dc == 0), stop=(dc == 7))

            def A2a(ti):
                X = xx[ti % 3]; XN = xnew[ti % 2]; r0 = ti * 128
                cp('act', wo_sb[:], pA[:])
                tt('pool', XN[:], X[:], wo_sb[:], ALU.add)
                dma(out[r0:r0 + 128, :], XN[:])
                act(junk2[:], XN[:], AF.Square, scale=1.0 / 32.0, accum=s4[:, 0:1])
                act(d4[:, 0:1], s4[:, 0:1], AF.Sqrt, bias=epsT[:, 0:1], scale=1.0)

            def A2b(ti):
                XN = xnew[ti % 2]; H2T = h2T[ti % 2]; r0 = ti * 128; SC = sc2[ti % 2]
                recip(r4[:, 0:1], d4[:, 0:1])
                ts('pool', mtmp[:], XN[:], r4[:, 0:1], 1.0, ALU.mult, ALU.mult)
                tt('pool', hn2[:], mtmp[:], gffn[:], ALU.mult)
                for dc in range(8):
                    tr(tp[:, dc * 128:(dc + 1) * 128], hn2[:, dc * 128:(dc + 1) * 128], idb[:])
                cp('act', H2T[:], tp[:].rearrange("p (c t) -> p c t", c=8))
                dma(HT[:, :, r0:r0 + 128].rearrange("c p t -> p c t"), H2T[:])
                for h in range(8):
                    for dc in range(8):
                        mm(pA[:, h * 128:(h + 1) * 128], Wqb[:, dc, h * 128:(h + 1) * 128], H2T[:, dc, :],
                           start=(dc == 0), stop=(dc == 7))
                cp('act', qT[:], pA[:].rearrange("p (h t) -> p h t", h=8))
                for h in range(8):
                    mm(pS[:, h * 256:(h + 1) * 256], qT[:, h, :], kbd[:, h, :])
                cp('act', SC[:], pS[:])

            def B1(ti):
                SC = sc2[ti % 2]
                segs = [(SC[:, g * 128:(g + 1) * 128], wk[:, g * 128:(g + 1) * 128]) for g in range(16)]
                for g in range(16):
                    vmax(sv[:, g, 0:8], segs[g][0])
                for g in range(16):
                    vmaxidx(si[:, g, 0:8], sv[:, g, 0:8], segs[g][0])
                    vmrep(segs[g][1], sv[:, g, 0:8], segs[g][0], NEG)
                for g in range(16):
                    vmax(sv[:, g, 8:16], segs[g][1])
                for g in range(16):
                    vmaxidx(si[:, g, 8:16], sv[:, g, 8:16], segs[g][1])
                cp('dve', sif[:], si[:])

            def B2a(ti):
                tt('dve', cand4, sv4[:, :, 0, :][:, :, :, None].to_broadcast([P, 8, 16, 16]),
                   sv4[:, :, 1, :][:, :, None, :].to_broadcast([P, 8, 16, 16]), ALU.add)
                for h in range(8):
                    vmax(best[:, h, 0:8], cand[:, h, :])
                for h in range(8):
                    vmaxidx(pos[:, h, 0:8], best[:, h, 0:8], cand[:, h, :])
                    vmrep(wk2[:, h, :], best[:, h, 0:8], cand[:, h, :], NEG)
                for h in range(8):
                    vmax(best[:, h, 8:16], wk2[:, h, :])
                for h in range(8):
                    vmaxidx(pos[:, h, 8:16], best[:, h, 8:16], wk2[:, h, :])
                tt('dve', dlt[:], best[:], best[:, :, 0:1].to_broadcast([P, 8, 16]), ALU.subtract)
                act(ex[:], dlt[:], AF.Exp)

            def B2b(ti):
                R = rt[ti % 2]; r0 = ti * 128
                tss(pa[:], pos[:], 4, ALU.logical_shift_right)
                tss(pb[:], pos[:], 15, ALU.bitwise_and)
                cp('dve', paf[:], pa[:]); cp('dve', pbf[:], pb[:])
                iob = io16[:][:, None, None, :].to_broadcast([P, 8, 16, 16])
                for which, pf in ((0, paf), (1, pbf)):
                    tt('dve', eq[:], iob, pf[:][:, :, :, None].to_broadcast([P, 8, 16, 16]), ALU.is_equal)
                    tt('dve', pr[:], eq[:], sif4[:, :, which, :][:, :, None, :].to_broadcast([P, 8, 16, 16]), ALU.mult)
                    red(IJG[:, which, :].rearrange("p (h k) -> p h k", h=8), pr[:])
                red(zz[:], ex[:])
                recip(rz[:], zz[:])
                G = IJG[:, 2, :].rearrange("p (h k) -> p h k", h=8)
                tt('dve', G, ex[:], rz[:][:, :, None].to_broadcast([P, 8, 16]), ALU.mult)
                for w3 in range(3):
                    tr(pR[:, w3 * 128:(w3 + 1) * 128], IJG[:, w3, :], idf[:])
                cp('act', R[:], pR[:, 0:384].rearrange("p (w t) -> p w t", w=3))
                dma(RT[:, :, r0:r0 + 128].rearrange("w p t -> p w t"), R[:])

            load(0); load(1); load(2)
            A0(0); A0(1)
            for dc in range(8):
                dma(wtmp[dc % 2][:], w_out[l, dc * 128:(dc + 1) * 128, :]); cp('act', Wo[:, dc, :], wtmp[dc % 2][:])
                dma(wtmp[2 + dc % 2][:], wq[l, dc * 128:(dc + 1) * 128, :]); cp('dve', Wqb[:, dc, :], wtmp[2 + dc % 2][:])
            dma(kbd[:], keysbd[l])
            dma(gout[:], out_norm[l:l + 1, :].to_broadcast([P, 1024]))
            dma(gffn[:], ffn_norm[l:l + 1, :].to_broadcast([P, 1024]))
            dma(idb[:], c_identb); dma(idf[:], c_identf); dma(io16[:], c_io16)
            A1(0); A2a(0); A2b(0)
            for ti in range(32):
                if ti + 3 < 32:
                    load(ti + 3)
                if ti + 1 < 32:
                    A1(ti + 1)
                B1(ti)
                if ti + 1 < 32:
                    A2a(ti + 1)
                if ti + 2 < 32:
                    A0(ti + 2)
                B2a(ti)
                if ti + 1 < 32:
                    A2b(ti + 1)
                B2b(ti)
            S.emit()

    def phase4(l):
        T = 256
        with ExitStack() as es:
            sb = lambda n, sh, dt: es.enter_context(nc.sbuf_tensor(nm(n), sh, dt))
            pst = lambda n, sh, dt: es.enter_context(nc.psum_tensor(nm(n), sh, dt))
            iota = sb("iota", [P, P], F32)
            Wbuf = [sb("Wbuf%d" % i, [P, T, 128], BF16) for i in range(2)]
            hT = [sb("hT%d" % i, [P, 8, T], BF16) for i in range(2)]
            r3 = [sb("r3_%d" % i, [P, 3, T], F32) for i in range(2)]
            NG = 8
            GI = [sb("GI%d" % i, [P, P], BF16) for i in range(NG)]
            OJ = [sb("OJ%d" % i, [P, P], BF16) for i in range(NG)]
            NBU = 8
            UV = [sb("UV%d" % i, [P, 2048], BF16) for i in range(NBU)]
            UTc = [UV[i][:, 0:1024] for i in range(NBU)]
            Vc = [UV[i][:, 1024:2048] for i in range(NBU)]
            NBV = NBU
            A = [sb("A%d" % i, [P, T], BF16) for i in range(3)]
            A2 = [sb("A2_%d" % i, [P, T], BF16) for i in range(3)]
            xx = [sb("xx%d" % i, [P, 1024], F32) for i in range(2)]
            xo = [sb("xo%d" % i, [P, 1024], F32) for i in range(2)]
            py = [pst("py%d" % i, [P, 512], F32) for i in range(4)]
            pSb = [pst("pS%d" % i, [P, 512], F32) for i in range(3)]
            pS = [pSb[i][:, 0:T] for i in range(3)]
            pW = [pst("pW%d" % i, [P, 512], F32) for i in range(1)]
            dma(iota[:], c_iota)
            ngroups = NT // T
            total = ngroups * 128

            def gload(gi):
                dma(hT[gi % 2][:], HT[:, :, gi * T:(gi + 1) * T].rearrange("c p t -> p c t"))
                dma(r3[gi % 2][:], RT[:, :, gi * T:(gi + 1) * T].rearrange("w p t -> p w t"))

            def uload(n):
                if n < total:
                    dma(UV[n % NBU][:], UVb[l, n % 128])

            def vload(n):
                pass

            wcs = [0]
            pend = []

            def wgen_dve(g, t):
                R = r3[g % 2]
                gi_t = GI[wcs[0] % NG]; oj_t = OJ[wcs[0] % NG]; wcs[0] += 1
                ts('dve', gi_t[:], iota[:], R[:, 0, t:t + 1], R[:, 2, t:t + 1], ALU.is_equal, ALU.mult)
                ts('dve', oj_t[:], iota[:], R[:, 1, t:t + 1], None, ALU.is_equal)
                pend.append((g, t, gi_t, oj_t))

            def wgen_pe():
                while pend:
                    g, t, gi_t, oj_t = pend.pop(0)
                    pw = pW[0]
                    mm(pw[:, (t % 4) * 128:(t % 4 + 1) * 128], oj_t[:], gi_t[:])
                    if t % 4 == 3:
                        cp('act' if (t // 4) % 2 == 0 else 'dve', Wbuf[g % 2][:, t - 3:t + 1, :],
                           pw[:].rearrange("p (t i) -> p t i", t=4))

            def s_mm(n):
                g = n // 128
                Uc = UTc[n % NBU]; H = hT[g % 2]
                for dc in range(8):
                    mm(pS[n % 3], Uc[:, dc * 128:(dc + 1) * 128], H[:, dc, :], start=(dc == 0), stop=(dc == 7))

            gload(0)
            for n in range(NBU - 1):
                uload(n)
            for n in range(NBV - 1):
                vload(n)
            for t in range(T):
                wgen_dve(0, t)
                if t % 2 == 1:
                    wgen_pe()
            s_mm(0); s_mm(1)
            for gi in range(ngroups):
                if gi + 1 < ngroups:
                    gload(gi + 1)
                for tt_ in range(2):
                    r0 = gi * T + tt_ * 128
                    dma(xx[tt_][:], out[r0:r0 + 128, :])
                for c in range(128):
                    n = gi * 128 + c
                    uload(n + NBU - 1)
                    vload(n + NBV - 1)
                    if n + 2 < total:
                        s_mm(n + 2)
                    Vv = Vc[n % NBV]
                    a = A[n % 3]; a2 = A2[n % 3]
                    act(a[:], pS[n % 3], AF.Gelu)
                    tt('dve', a2[:], a[:], Wbuf[gi % 2][:, :, c], ALU.mult)
                    wgen_pe()
                    if gi + 1 < ngroups:
                        wgen_dve(gi + 1, 2 * c); wgen_dve(gi + 1, 2 * c + 1)
                    for tt_ in range(2):
                        for hh in range(2):
                            mm(py[tt_ * 2 + hh][:], a2[:, tt_ * 128:(tt_ + 1) * 128], Vv[:, hh * 512:(hh + 1) * 512],
                               start=(c == 0), stop=(c == 127))
                wgen_pe()
                for tt_ in range(2):
                    r0 = gi * T + tt_ * 128
                    for hh in range(2):
                        tt('dve', xo[tt_][:, hh * 512:(hh + 1) * 512], xx[tt_][:, hh * 512:(hh + 1) * 512], py[tt_ * 2 + hh][:], ALU.add)
                    dma(out[r0:r0 + 128, :], xo[tt_][:])
            S.emit()

    stages = []
    for l in range(2):
        xs = x if l == 0 else out
        stages.append(('p1_%d' % l, lambda l=l, xs=xs: phase1(l, xs)))
        stages.append(('p2_%d' % l, lambda l=l: phase2(l)))
        stages.append(('p3_%d' % l, lambda l=l, xs=xs: phase3(l, xs)))
        stages.append(('p4_%d' % l, lambda l=l: phase4(l)))
    for name, fn in stages:
        fn()
        if stop_after == name:
            break
    return nc


def _consts():
    f32 = np.float32
    bf = ml_dtypes.bfloat16
    c = {}
    c["c_identb"] = np.eye(P, dtype=f32).astype(bf)
    c["c_identf"] = np.eye(P, dtype=f32)
    pos = np.arange(2048, dtype=f32)

    def invf(dim):
        return (f32(10000.0) ** (-(np.arange(0, dim, 2, dtype=f32) / f32(dim)))).astype(f32)
    r = np.arange(P)
    d = r % 64
    f = invf(64)[d % 32]
    ang = (pos[None, :] * f[:, None]).astype(f32)
    sgn = np.where(d < 32, -1.0, 1.0).astype(f32)
    c["c_ct1"] = np.cos(ang).astype(f32)
    c["c_st1"] = (np.sin(ang) * sgn[:, None]).astype(f32)
    e = d % 32
    which = d // 32
    fa = invf(32)[e % 16]
    rowp = np.floor(pos / 64).astype(f32); colp = (pos % 64).astype(f32)
    pv_ = np.where(which[:, None] == 0, rowp[None, :], colp[None, :]).astype(f32)
    anga = (pv_ * fa[:, None]).astype(f32)
    sgna = np.where(e < 16, -1.0, 1.0).astype(f32)
    c["c_cta"] = np.cos(anga).astype(f32)
    c["c_sta"] = (np.sin(anga) * sgna[:, None]).astype(f32)
    p1 = np.where(d < 32, r + 32, r - 32)
    pa = np.where(e < 16, r + 16, r - 16)
    m1 = np.zeros((P, P), f32); m1[p1, r] = 1.0
    ma = np.zeros((P, P), f32); ma[pa, r] = 1.0
    c["c_psw1"] = m1.astype(bf); c["c_pswa"] = ma.astype(bf)
    c["c_bones"] = ((r[:, None] // 64) == (r[None, :] // 64)).astype(f32).astype(bf)
    cc = np.arange(3968)
    dd = cc[None, :] - 1920 - r[:, None]
    ad = np.abs(dd)
    fb = (ad <= 64).astype(f32) + ((dd % 4 == 0) & (ad <= 256)).astype(f32) + ((dd % 16 == 0) & (ad <= 1024)).astype(f32)
    c["c_tb"] = np.where(fb > 0, 8.0 * np.log(np.maximum(fb, 1.0)), -30000.0).astype(f32).astype(bf)
    c["c_tc"] = np.where(ad <= 128, 0.0, -30000.0).astype(f32).astype(bf)
    c["c_iota"] = np.tile(np.arange(P, dtype=f32)[None, :], (P, 1))
    c["c_io16"] = np.tile(np.arange(16, dtype=f32)[None, :], (P, 1))
    return c


def _shared_inputs(attn_norm, w_in, qk_gain, sink_logits, out_norm, w_out, ffn_norm, peer_wq, peer_keys, peer_u, peer_v):
    f32 = np.float32
    m = dict(attn_norm=np.ascontiguousarray(attn_norm, f32), out_norm=np.ascontiguousarray(out_norm, f32),
             ffn_norm=np.ascontiguousarray(ffn_norm, f32), w_in=np.ascontiguousarray(w_in, f32),
             w_out=np.ascontiguousarray(w_out, f32), wq=np.ascontiguousarray(peer_wq, f32),
             pu=np.ascontiguousarray(peer_u, f32), pv=np.ascontiguousarray(peer_v, f32))
    sel = [(0, 0)] * 3 + [(0, 1)] + [(1, 0)] * 3 + [(1, 1)] * 3 + [(2, 0)] * 2 + [(2, 1)]
    gc = np.zeros((2, P, 13), f32)
    for l in range(2):
        for ci, (mi, qi) in enumerate(sel):
            gc[l, :, ci] = np.tile(np.asarray(qk_gain[l, mi, qi], f32), 2)
    m["gaincol"] = gc
    m["sink"] = np.ascontiguousarray(np.asarray(sink_logits, f32).reshape(1, 8))
    kb = np.zeros((2, P, 8, 256), f32)
    pk = np.asarray(peer_keys, f32)
    for p in range(2):
        kb[:, p * 64:(p + 1) * 64, :, p * 128:(p + 1) * 128] = pk[:, :, p].transpose(0, 3, 1, 2)
    m["keysbd"] = kb
    m.update(_consts())
    return m


def kernel(x, attn_norm, w_in, qk_gain, sink_logits, out_norm, w_out, ffn_norm, peer_wq, peer_keys, peer_u, peer_v):
    x = np.asarray(x, np.float32)
    shared = _shared_inputs(attn_norm, w_in, qk_gain, sink_logits, out_norm, w_out, ffn_norm,
                            peer_wq, peer_keys, peer_u, peer_v)
    nc = build()
    in_maps = []
    for c in range(8):
        m = dict(shared)
        m["x"] = np.ascontiguousarray(x[2 * c:2 * c + 2].reshape(NT, 1024))
        in_maps.append(m)
    res = run_bass_kernel_spmd(nc, in_maps, core_ids=list(range(8)))
    outs = [np.asarray(r["out"], np.float32).reshape(2, 2048, 1024) for r in res.results]
    return np.concatenate(outs, axis=0)
```

```python
import numpy as np
import ml_dtypes
from contextlib import ExitStack
import concourse.bass as bass
import concourse.mybir as mybir
from concourse.bass_utils import run_bass_kernel_spmd

F32 = mybir.dt.float32
BF16 = mybir.dt.bfloat16
U32 = mybir.dt.uint32
ALU = mybir.AluOpType
AF = mybir.ActivationFunctionType
AX = mybir.AxisListType
P = 128
NT = 4096
EPS = 1e-6
NEG = -1e30

QK_CHUNKS = [(0, 1), (128, 1), (256, 1), (384, 1),
             (640, 0), (768, 0), (896, 0), (1024, 0), (1152, 0), (1280, 0),
             (1792, 0), (1920, 0), (2048, 0)]
V_RANGES = [(512, 128, 0), (1408, 384, 128), (2176, 128, 512)]


class Sched:
    ENGS = ('sp', 'act', 'dve', 'pool', 'pe')

    def __init__(s, nc, ndma=72):
        s.nc = nc
        s.sems, s.cnt, s.esem, s.dpool = [], [], {}, []
        for e in s.ENGS:
            s.esem[e] = len(s.sems); s.sems.append(nc.alloc_semaphore('e_' + e)); s.cnt.append(0)
        for i in range(ndma):
            s.dpool.append(len(s.sems)); s.sems.append(nc.alloc_semaphore('d%d' % i)); s.cnt.append(0)
        s.seen = {e: {} for e in s.ENGS}
        s.new_phase()

    def new_phase(s):
        s.stream = {e: [] for e in s.ENGS}
        s.trk, s.dsem, s.dnext = {}, {}, 0

    @staticmethod
    def _names(aps):
        out = []
        for a in aps:
            if a is None or isinstance(a, (int, float)):
                continue
            if isinstance(a, str):
                out.append(a); continue
            if str(a.space) == 'DRAM':
                continue
            out.append(a.tensor.name)
        return out

    def op(s, eng, fn, reads=(), writes=(), dma_on=None, sreads=()):
        rn, wn = s._names(reads) + s._names(sreads), s._names(writes)
        own = s.esem[eng]
        raw, war = {}, {}
        for n in rn:
            t = s.trk.get(n)
            if t:
                for k, v in t[0].items():
                    raw[k] = max(raw.get(k, 0), v)
        for n in wn:
            t = s.trk.get(n)
            if t:
                for d in t:
                    for k, v in d.items():
                        war[k] = max(war.get(k, 0), v)
        deps = dict(raw)
        if eng == 'pe':
            deps.pop(own, None)
        for k, v in war.items():
            if k == own and dma_on is None:
                continue
            deps[k] = max(deps.get(k, 0), v)
        waits = []
        for k, v in deps.items():
            if s.seen[eng].get(k, 0) < v:
                waits.append((k, v)); s.seen[eng][k] = v
        if dma_on is None:
            k = own; inc = 1
        else:
            name = dma_on.tensor.name
            if name not in s.dsem:
                s.dsem[name] = s.dpool[s.dnext]; s.dnext += 1
            k = s.dsem[name]; inc = 16
        s.cnt[k] += inc
        v = s.cnt[k]
        for n in rn:
            s.trk.setdefault(n, ({}, {}))[1][k] = v
        for n in wn:
            s.trk.setdefault(n, ({}, {}))[0][k] = v
        s.stream[eng].append((waits, fn, k, inc))

    def barrier(s):
        for e in s.ENGS:
            waits = []
            for k in range(len(s.sems)):
                if s.cnt[k] > 0 and s.seen[e].get(k, 0) < s.cnt[k]:
                    waits.append((k, s.cnt[k])); s.seen[e][k] = s.cnt[k]
            s.stream[e].append((waits, None, 0, 0))

    def emit(s):
        nc = s.nc
        s.barrier()
        with nc.Block() as blk:
            def mk(name):
                def body(e):
                    for waits, fn, k, inc in s.stream[name]:
                        for (sk, v) in waits:
                            e.wait_ge(s.sems[sk], v)
                        if fn is not None:
                            fn(e).then_inc(s.sems[k], inc)
                return body
            blk.sync(mk('sp')); blk.scalar(mk('act')); blk.vector(mk('dve'))
            blk.gpsimd(mk('pool')); blk.tensor(mk('pe'))
        s.new_phase()


def build(stop_after=None, dbg=False):
    nc = bass.Bass("TRN2", target_bir_lowering=False)

    def din(name, shape, dt=F32):
        return nc.dram_tensor(name, list(shape), dt, kind="ExternalInput").ap()

    def dscr(name, shape, dt, big=False):
        kind = "ExternalOutput" if (dbg and not big) else "Internal"
        return nc.dram_tensor(name, list(shape), dt, kind=kind).ap()

    x = din("x", [NT, 1024])
    attn_norm = din("attn_norm", [2, 1024]); out_norm = din("out_norm", [2, 1024]); ffn_norm = din("ffn_norm", [2, 1024])
    w_in = din("w_in", [2, 1024, 2304]); w_out = din("w_out", [2, 1024, 1024]); wq = din("wq", [2, 1024, 1024])
    gaincol = din("gaincol", [2, P, 13]); sink = din("sink", [1, 8]); keysbd = din("keysbd", [2, P, 8, 256])
    pu = din("pu", [2, 16384, 1024]); pv = din("pv", [2, 16384, 1024])
    c_identb = din("c_identb", [P, P], BF16); c_identf = din("c_identf", [P, P])
    c_ct1 = din("c_ct1", [P, 2048]); c_st1 = din("c_st1", [P, 2048]); c_cta = din("c_cta", [P, 2048]); c_sta = din("c_sta", [P, 2048])
    c_psw1 = din("c_psw1", [P, P], BF16); c_pswa = din("c_pswa", [P, P], BF16); c_bones = din("c_bones", [P, P], BF16)
    c_tb = din("c_tb", [P, 3968], BF16); c_tc = din("c_tc", [P, 3968], BF16)
    c_iota = din("c_iota", [P, P]); c_io16 = din("c_io16", [P, 16])
    out = nc.dram_tensor("out", [NT, 1024], F32, kind="ExternalOutput").ap()

    UVb = dscr("UVb", [2, 128, P, 2048], BF16, big=True)
    QKT = dscr("QKT", [13, P, NT], BF16)
    Vtok = dscr("Vtok", [NT, 640], BF16)
    MIX = dscr("MIX", [NT, 1024], F32)
    HT = dscr("HT", [8, P, NT], BF16)
    RT = dscr("RT", [3, P, NT], F32)

    S = Sched(nc)
    uid = [0]

    def nm(n):
        uid[0] += 1
        return '%s_%d' % (n, uid[0])

    def mm(o, lhsT, rhs, start=True, stop=True, okey=None):
        S.op('pe', lambda e: e.matmul(o, lhsT, rhs, start=start, stop=stop), [lhsT, rhs], [okey or o])

    def tr(o, i, ident):
        S.op('pe', lambda e: e.transpose(o, i, ident), [i, ident], [o])

    def act(o, i, func, bias=None, scale=None, accum=None, ikey=None):
        kw = {}
        if bias is not None: kw['bias'] = bias
        if scale is not None: kw['scale'] = scale
        if accum is not None: kw['accum_out'] = accum
        S.op('act', lambda e: e.activation(o, i, func, **kw), [ikey or i], [o, accum], sreads=[bias, scale])

    def tt(eng, o, a, b, op):
        en = {'dve': 'dve', 'pool': 'pool'}[eng]
        S.op(en, lambda e: e.tensor_tensor(o, a, b, op), [a, b], [o])

    def ts(eng, o, a, s1, s2, op0, op1=None):
        if op1 is None:
            S.op(eng, lambda e: e.tensor_scalar(o, a, s1, s2, op0), [a], [o], sreads=[s1, s2])
        else:
            S.op(eng, lambda e: e.tensor_scalar(o, a, s1, s2, op0, op1), [a], [o], sreads=[s1, s2])

    def stt(o, a, sc, b, op0, op1):
        S.op('dve', lambda e: e.scalar_tensor_tensor(o, a, sc, b, op0, op1), [a, b], [o], sreads=[sc])

    def cp(eng, o, i):
        if eng == 'act':
            S.op('act', lambda e: e.copy(o, i), [i], [o])
        else:
            S.op(eng, lambda e: e.tensor_copy(o, i), [i], [o])

    def recip(o, i):
        S.op('dve', lambda e: e.reciprocal(o, i), [i], [o])

    def recip_fast(o, i):
        S.op('dve', lambda e: e.reciprocal_approx_fast(o, i), [i], [o])

    def red(o, i, op=ALU.add):
        S.op('dve', lambda e: e.tensor_reduce(o, i, AX.X, op), [i], [o])

    def memset(eng, o, val):
        S.op(eng, lambda e: e.memset(o, val), [], [o])

    def dma(o, i, eng='sp'):
        sbside = o if str(o.space) != 'DRAM' else i
        S.op(eng, lambda e: e.dma_start(out=o, in_=i), [i], [o], dma_on=sbside)

    def vmax(o, i):
        S.op('dve', lambda e: e.max(o, i), [i], [o])

    def vmaxidx(o, mx, vals):
        S.op('dve', lambda e: e.max_index(o, mx, vals), [mx, vals], [o])

    def vmrep(o, rep, vals, imm):
        S.op('dve', lambda e: e.match_replace(o, rep, vals, imm), [rep, vals], [o])

    def tss(o, i, sc, op):
        S.op('dve', lambda e: e.tensor_single_scalar(o, i, sc, op), [i], [o])

    def prepass():
        with ExitStack() as es:
            sb = lambda n, sh, dt: es.enter_context(nc.sbuf_tensor(nm(n), sh, dt))
            pst = lambda n, sh, dt: es.enter_context(nc.psum_tensor(nm(n), sh, dt))
            uf = [sb("uf%d" % i, [P, 1024], F32) for i in range(3)]
            vf = [sb("vf%d" % i, [P, 1024], F32) for i in range(3)]
            ub = [sb("ub%d" % i, [P, 1024], BF16) for i in range(2)]
            vb = [sb("vb%d" % i, [P, 1024], BF16) for i in range(2)]
            ut = [sb("ut%d" % i, [P, 1024], BF16) for i in range(2)]
            pt = [pst("pt%d" % i, [P, 1024], BF16) for i in range(2)]
            idb = sb("idb", [P, P], BF16)
            dma(idb[:], c_identb)
            blocks = [(l, c) for l in range(2) for c in range(128)]

            def load(i):
                l, c = blocks[i]
                dma(uf[i % 3][:], pu[l, c * 128:(c + 1) * 128, :])
                dma(vf[i % 3][:], pv[l, c * 128:(c + 1) * 128, :])
            load(0); load(1)
            for i, (l, c) in enumerate(blocks):
                if i + 2 < len(blocks):
                    load(i + 2)
                cp('pool', ub[i % 2][:], uf[i % 3][:])
                cp('act', vb[i % 2][:], vf[i % 3][:])
                for dc in range(8):
                    tr(pt[i % 2][:, dc * 128:(dc + 1) * 128], ub[i % 2][:, dc * 128:(dc + 1) * 128], idb[:])
                cp('dve', ut[i % 2][:], pt[i % 2][:])
                dma(UVb[l, c, :, 0:1024], ut[i % 2][:])
                dma(UVb[l, c, :, 1024:2048], vb[i % 2][:])
            S.emit()

    def phase1(l, xsrc):
        with ExitStack() as es:
            sb = lambda n, sh, dt: es.enter_context(nc.sbuf_tensor(nm(n), sh, dt))
            pst = lambda n, sh, dt: es.enter_context(nc.psum_tensor(nm(n), sh, dt))
            Wb = sb("Wb", [P, 8, 2304], BF16)
            wtmp = [sb("wtmp%d" % i, [P, 2304], F32) for i in range(2)]
            gbc = sb("gbc", [P, 1024], F32)
            ct1 = sb("ct1", [P, 2048], F32); st1 = sb("st1", [P, 2048], F32)
            cta = sb("cta", [P, 2048], F32); sta = sb("sta", [P, 2048], F32)
            gcol = sb("gcol", [P, 13], F32)
            bones = sb("bones", [P, P], BF16); psw1 = sb("psw1", [P, P], BF16); pswa = sb("pswa", [P, P], BF16)
            idb = sb("idb", [P, P], BF16)
            epsT = sb("epsT", [P, 1], F32)
            xt = [sb("xt%d" % i, [P, 4, 1024], F32) for i in range(2)]
            junk = sb("junk", [P, 1024], BF16)
            ssq = sb("ssq", [P, 4], F32); sd4 = sb("sd4", [P, 4], F32); rstd = sb("rstd", [P, 4], F32)
            hn = [sb("hn%d" % i, [P, 1024], BF16) for i in range(2)]
            hnT = [sb("hnT%d" % i, [P, 8, 512], BF16) for i in range(2)]
            vtok = [sb("vtok%d" % i, [P, 640], BF16) for i in range(2)]
            sq = [sb("sq%d" % i, [P, 512], BF16) for i in range(2)]
            sdt = [sb("sdt%d" % i, [P, 512], F32) for i in range(2)]
            rs = [sb("rs%d" % i, [P, 512], F32) for i in range(2)]
            xn = [sb("xn%d" % i, [P, 512], BF16) for i in range(2)]
            t1 = [sb("t1_%d" % i, [P, 512], F32) for i in range(2)]
            t2 = [sb("t2_%d" % i, [P, 512], F32) for i in range(2)]
            qk = [sb("qk%d" % i, [P, 512], BF16) for i in range(2)]
            tp = pst("tp", [P, 1024], BF16)
            pvA = pst("pvA", [P, 512], F32); pvB = pst("pvB", [P, 512], F32)
            pq = [pst("pq%d" % i, [P, 512], F32) for i in range(3)]
            pss = pst("pss", [P, 512], F32)
            psw = [pst("psw%d" % i, [P, 512], F32) for i in range(1)]

            def load(tb):
                dma(xt[tb % 2][:], xsrc[tb * 512:(tb + 1) * 512, :].rearrange("(t p) d -> p t d", p=P))
            load(0)
            memset('dve', epsT[:], EPS)
            dma(gbc[:], attn_norm[l:l + 1, :].to_broadcast([P, 1024]))
            dma(idb[:], c_identb)
            dma(gcol[:], gaincol[l]); dma(bones[:], c_bones); dma(psw1[:], c_psw1); dma(pswa[:], c_pswa)
            for dc in range(8):
                dma(wtmp[dc % 2][:], w_in[l, dc * 128:(dc + 1) * 128, :])
                cp('act' if dc % 2 else 'dve', Wb[:, dc, :], wtmp[dc % 2][:])
            dma(ct1[:], c_ct1); dma(st1[:], c_st1); dma(cta[:], c_cta); dma(sta[:], c_sta)

            cc = 0
            for tb in range(8):
                if tb + 1 < 8:
                    load(tb + 1)
                X = xt[tb % 2]; HTt = hnT[tb % 2]
                pos0 = (tb % 4) * 512
                for t in range(4):
                    act(junk[:], X[:, t, :], AF.Square, accum=ssq[:, t:t + 1])
                act(sd4[:], ssq[:], AF.Sqrt, bias=epsT[:, 0:1], scale=1.0 / 1024)
                recip(rstd[:], sd4[:])
                for t in range(4):
                    H = hn[t % 2]
                    stt(H[:], X[:, t, :], rstd[:, t:t + 1], gbc[:], ALU.mult, ALU.mult)
                    for dc in range(8):
                        tr(tp[:, dc * 128:(dc + 1) * 128], H[:, dc * 128:(dc + 1) * 128], idb[:])
                    cp('act', HTt[:, :, t * 128:(t + 1) * 128], tp[:].rearrange("p (c t) -> p c t", c=8))
                for t in range(4):
                    VT = vtok[t % 2]
                    for (c0, w, oc) in V_RANGES:
                        dst = pvA[:, oc:oc + w] if oc < 512 else pvB[:, 0:w]
                        for dc in range(8):
                            mm(dst, HTt[:, dc, t * 128:(t + 1) * 128], Wb[:, dc, c0:c0 + w], start=(dc == 0), stop=(dc == 7))
                    cp('act', VT[:, 0:512], pvA[:])
                    cp('dve', VT[:, 512:640], pvB[:, 0:128])
                    r0 = tb * 512 + t * 128
                    dma(Vtok[r0:r0 + 128, :], VT[:])
                def s1(ci):
                    c0, ax = QK_CHUNKS[ci]; k = ci % 2; k3 = ci % 3
                    for dc in range(8):
                        mm(pq[k3][:], Wb[:, dc, c0:c0 + 128], HTt[:, dc, :], start=(dc == 0), stop=(dc == 7))
                    act(sq[k][:], pq[k3][:], AF.Square)

                def s2(ci):
                    k = ci % 2; k3 = ci % 3
                    mm(pss[:], bones[:], sq[k][:])
                    act(sdt[k][:], pss[:], AF.Ln, bias=epsT[:, 0:1], scale=1.0 / 64)
                    act(rs[k][:], sdt[k][:], AF.Exp, scale=-0.5)
                    stt(xn[k][:], pq[k3][:], gcol[:, ci:ci + 1], rs[k][:], ALU.mult, ALU.mult)

                def s3(ci):
                    c0, ax = QK_CHUNKS[ci]; k = ci % 2
                    CT, ST, PSW = (cta, sta, pswa) if ax else (ct1, st1, psw1)
                    mm(psw[0][:], PSW[:], xn[k][:])
                    tt('pool', t1[k][:], xn[k][:], CT[:, pos0:pos0 + 512], ALU.mult)
                    tt('dve', t2[k][:], psw[0][:], ST[:, pos0:pos0 + 512], ALU.mult)
                    tt('pool', qk[k][:], t1[k][:], t2[k][:], ALU.add)
                    dma(QKT[ci, :, tb * 512:(tb + 1) * 512], qk[k][:])
                NCH = len(QK_CHUNKS)
                s1(0); s1(1); s2(0)
                for ci in range(NCH):
                    if ci + 2 < NCH:
                        s1(ci + 2)
                    if ci + 1 < NCH:
                        s2(ci + 1)
                    s3(ci)
            S.emit()

    def phase2(l):
        with ExitStack() as es:
            sb = lambda n, sh, dt: es.enter_context(nc.sbuf_tensor(nm(n), sh, dt))
            pst = lambda n, sh, dt: es.enter_context(nc.psum_tensor(nm(n), sh, dt))
            KT = [sb("KT%d" % i, [P, 2048], BF16) for i in range(2)]
            QTh = [[sb("QT%d_%d" % (h_, i), [P, 2048], BF16) for i in range(2)] for h_ in range(2)]
            Vp = [sb("Vp%d" % i, [P, 16, 65], BF16) for i in range(2)]
            tbm = sb("tbm", [P, 3968], BF16); tcm = sb("tcm", [P, 3968], BF16)
            mix = sb("mixsb", [P, 16, 1024], F32)
            NPS = 5
            pe_ = [sb("pe%d" % i, [P, 512], BF16) for i in range(NPS)]
            snk = sb("snk", [P, 8], F32); esk = sb("esk", [P, 8], F32)
            lt = [sb("lt%d" % i, [P, 1], F32) for i in range(4)]
            rl = [sb("rl%d" % i, [P, 1], F32) for i in range(4)]
            psc = [pst("psc%d" % i, [P, 512], F32) for i in range(NPS)]
            pot = [pst("pot%d" % i, [P, 512], F32) for i in range(1)]
            ptr = pst("ptr", [P, 512], F32)
            ot_sb = [sb("ot_sb%d" % i, [P, 512], F32) for i in range(2)]
            lt4 = sb("lt4", [P, 4], F32); rl4 = sb("rl4", [P, 4], F32)
            idf = sb("idf", [P, P], F32)
            dma(idf[:], c_identf)
            uf = [sb("uf%d" % i, [P, 1024], F32) for i in range(3)]
            vf = [sb("vf%d" % i, [P, 1024], F32) for i in range(3)]
            ub = [sb("ub%d" % i, [P, 1024], BF16) for i in range(2)]
            vb = [sb("vb%d" % i, [P, 1024], BF16) for i in range(2)]
            ut = [sb("ut%d" % i, [P, 1024], BF16) for i in range(2)]
            ptp = pst("ptp", [P, 1024], BF16)
            idb = sb("idb", [P, P], BF16)
            dma(idb[:], c_identb)

            def pre_load(i):
                if i < 128:
                    dma(uf[i % 3][:], pu[l, i * 128:(i + 1) * 128, :])
                    dma(vf[i % 3][:], pv[l, i * 128:(i + 1) * 128, :])

            def pre_cast(i):
                cp('pool', ub[i % 2][:], uf[i % 3][:])
                cp('pool', vb[i % 2][:], vf[i % 3][:])
                dma(UVb[l, i, :, 1024:2048], vb[i % 2][:])

            def pre_tr(i):
                if i < 0:
                    return
                for dc in range(8):
                    tr(ptp[:, dc * 128:(dc + 1) * 128], ub[i % 2][:, dc * 128:(dc + 1) * 128], idb[:])
                cp('dve', ut[i % 2][:], ptp[:])
                dma(UVb[l, i, :, 0:1024], ut[i % 2][:])
            pre_load(0); pre_load(1)

            dma(tbm[:], c_tb); dma(tcm[:], c_tc)
            dma(snk[:], sink[0:1, :].to_broadcast([P, 8]))
            m8 = sb("m8", [P, 1], F32)
            memset('dve', m8[:], -8.0)
            act(esk[:], snk[:], AF.Exp, bias=m8[:, 0:1])
            for i in range(2):
                memset('pool', Vp[i][:], 1.0)
                for h_ in range(2):
                    memset('pool', QTh[h_][i][:], 0.0)

            groups = []
            for g in range(2):
                groups.append(('A', 3, g, g * 64, [(h // 2, h % 2, h * 64, None) for h in range(3 * g, 3 * g + 3)]))
            for h in range(6):
                groups.append(('B', 7 + h // 2, h % 2, 128 + h * 64, [(4 + h // 2, h % 2, 384 + h * 64, None)]))
            for g in range(2):
                groups.append(('C', 12, g, 512 + g * 64, [(10 + h // 2, h % 2, 768 + h * 64, h) for h in range(2 * g, 2 * g + 2)]))

            units = []
            kvc = 0; qc = 0
            for b in range(2):
                for (kind, kch, kh, vcol, qheads) in groups:
                    for qi, (qch, qh, mcol, sidx) in enumerate(qheads):
                        for qb in range(4):
                            units.append(dict(b=b, kind=kind, kch=kch, kh=kh, vcol=vcol, qch=qch, qh=qh, mcol=mcol,
                                              sidx=sidx, qb=qb, kvi=kvc, qi=qc, newkv=(qi == 0 and qb == 0), newq=(qb == 0)))
                        qc += 1
                    kvc += 1
            for i_, u in enumerate(units):
                u['lastb'] = (i_ + 1 == len(units)) or (units[i_ + 1]['b'] != u['b'])
            ecs = [0]

            def qk_mm(u, i):
                kt = u['kts'][i]; K = u['K']; Q = u['Q']; qb = u['qb']; sl = u['slots'][i]
                if u['kind'] == 'A':
                    mm(psc[sl][:], K[:, kt * 128:(kt + 1) * 128], Q[:, qb * 512:(qb + 1) * 512])
                else:
                    M = tbm if u['kind'] == 'B' else tcm
                    cst = (qb * 4 - kt + 15) * 128
                    mm(psc[sl][:], K[:, kt * 128:(kt + 1) * 128], Q[:, qb * 512:(qb + 1) * 512], start=True, stop=False)
                    mm(psc[sl][:], idb[:], M[:, cst:cst + 512], start=False, stop=True)

            def start_unit(u):
                t0 = u['b'] * 2048; kh = u['kh']
                u['K'] = KT[u['kvi'] % 2]; u['V'] = Vp[u['kvi'] % 2]; u['Q'] = QTh[kh][u['qi'] % 2]
                if u['newkv']:
                    dma(u['K'][:], QKT[u['kch'], :, t0:t0 + 2048])
                    dma(u['V'][:, :, 0:64], Vtok[t0:t0 + 2048, u['vcol']:u['vcol'] + 64].rearrange("(t p) c -> p t c", p=P))
                if u['newq']:
                    dma(u['Q'][kh * 64:(kh + 1) * 64, :], QKT[u['qch'], u['qh'] * 64:(u['qh'] + 1) * 64, t0:t0 + 2048])
                qt0 = u['qb'] * 4
                rad = {'A': 99, 'B': 8, 'C': 1}[u['kind']]
                kts = [kt for kt in range(16) if any(abs(qt0 + j - kt) <= rad for j in range(4))]
                u['kts'] = kts
                u['slots'] = [(ecs[0] + i) % NPS for i in range(len(kts))]
                ecs[0] += len(kts)
                for i in range(min(NPS - 1, len(kts))):
                    qk_mm(u, i)

            start_unit(units[0])
            for unit, u in enumerate(units):
                pre_load(unit + 2)
                pre_tr(unit - 1)
                kts = u['kts']; nk = len(kts); V = u['V']; qt0 = u['qb'] * 4; mcol = u['mcol']; sidx = u['sidx']
                for i, kt in enumerate(kts):
                    if i + NPS - 1 < nk:
                        qk_mm(u, i + NPS - 1)
                    ps_ = psc[u['slots'][i]]; pe = pe_[u['slots'][i]]
                    act(pe[:], ps_[:], AF.Exp, bias=m8[:, 0:1], scale=0.125)
                    mm(pot[0][0:65, :], V[:, kt, :], pe[:], start=(i == 0), stop=(i == nk - 1))
                if unit + 1 < len(units):
                    start_unit(units[unit + 1])
                osb = ot_sb[unit % 2]
                cp('dve', osb[0:65, :], pot[0][0:65, :])
                for j in range(4):
                    tr(ptr[:, j * 65:(j + 1) * 65], osb[0:65, j * 128:(j + 1) * 128], idf[0:65, 0:65])
                ptr3 = ptr[:, 0:260].rearrange("p (j c) -> p j c", j=4)
                if sidx is not None:
                    ts('dve', lt4[:], ptr3[:, :, 64], esk[:, l * 4 + sidx:l * 4 + sidx + 1], None, ALU.add)
                    recip(rl4[:], lt4[:])
                else:
                    recip(rl4[:], ptr3[:, :, 64])
                tt('dve', mix[:, qt0:qt0 + 4, mcol:mcol + 64], ptr3[:, :, 0:64],
                   rl4[:][:, :, None].to_broadcast([P, 4, 64]), ALU.mult)
                pre_cast(unit)
                if u['lastb']:
                    t0 = u['b'] * 2048
                    dma(MIX[t0:t0 + 2048, :].rearrange("(t p) d -> p t d", p=P), mix[:])
            pre_tr(127)
            S.emit()

    def phase3(l, xsrc):
        with ExitStack() as es:
            sb = lambda n, sh, dt: es.enter_context(nc.sbuf_tensor(nm(n), sh, dt))
            pst = lambda n, sh, dt: es.enter_context(nc.psum_tensor(nm(n), sh, dt))
            Wo = sb("Wo", [P, 8, 1024], BF16); Wqb = sb("Wqb", [P, 8, 1024], BF16)
            wtmp = [sb("wtmp%d" % i, [P, 1024], F32) for i in range(4)]
            kbd = sb("kbd", [P, 8, 256], F32)
            gout = sb("gout", [P, 1024], F32); gffn = sb("gffn", [P, 1024], F32)
            idb = sb("idb", [P, P], BF16); idf = sb("idf", [P, P], F32)
            io16 = sb("io16", [P, 16], F32)
            epsT = sb("epsT", [P, 1], F32)
            mx = [sb("mx%d" % i, [P, 1024], F32) for i in range(3)]
            xx = [sb("xx%d" % i, [P, 1024], F32) for i in range(3)]
            junk = sb("junk", [P, 1024], BF16)
            s3b = [sb("s3b%d" % i, [P, 4], F32) for i in range(2)]; d3b = [sb("d3b%d" % i, [P, 4], F32) for i in range(2)]
            s3 = sb("s3", [P, 4], F32); d3 = sb("d3", [P, 4], F32); r3 = sb("r3", [P, 4], F32)
            mixn = sb("mixn", [P, 1024], BF16)
            mixT = sb("mixT", [P, 8, 128], BF16)
            xnew = [sb("xnew%d" % i, [P, 1024], F32) for i in range(2)]
            hn2 = sb("hn2", [P, 1024], BF16)
            h2T = [sb("h2T%d" % i, [P, 8, 128], BF16) for i in range(2)]
            qT = sb("qT", [P, 8, 128], F32)
            sc2 = [sb("sc%d" % i, [P, 2048], F32) for i in range(2)]
            junk2 = sb("junk2", [P, 1024], BF16)
            mtmp = sb("mtmp", [P, 1024], F32); wo_sb = sb("wo_sb", [P, 1024], F32)
            s4 = sb("s4", [P, 1], F32); d4 = sb("d4", [P, 1], F32); r4 = sb("r4", [P, 1], F32)
            wk = sb("wk", [P, 2048], F32)
            sv = sb("sv", [P, 16, 16], F32); si = sb("si", [P, 16, 16], U32); sif = sb("sif", [P, 16, 16], F32)
            cand = sb("cand", [P, 8, 256], F32); wk2 = sb("wk2", [P, 8, 256], F32)
            best = sb("best", [P, 8, 16], F32); pos = sb("pos", [P, 8, 16], U32)
            dlt = sb("dlt", [P, 8, 16], F32); ex = sb("ex", [P, 8, 16], F32)
            zz = sb("zz", [P, 8], F32); rz = sb("rz", [P, 8], F32)
            pa = sb("pa", [P, 8, 16], U32); pb = sb("pb", [P, 8, 16], U32)
            paf = sb("paf", [P, 8, 16], F32); pbf = sb("pbf", [P, 8, 16], F32)
            eq = sb("eq", [P, 8, 16, 16], F32); pr = sb("pr", [P, 8, 16, 16], F32)
            eq2 = sb("eq2", [P, 8, 16, 16], F32); pr2 = sb("pr2", [P, 8, 16, 16], F32)
            IJG = sb("IJG", [P, 3, 128], F32)
            rt = [sb("rt%d" % i, [P, 3, 128], F32) for i in range(2)]
            tp = pst("tp", [P, 1024], BF16)
            pA = pst("pA", [P, 1024], F32)
            pS = pst("pS", [P, 2048], F32)
            pR = pst("pR", [P, 512], F32)

            memset('dve', epsT[:], EPS)

            def load(ti):
                dma(mx[ti % 3][:], MIX[ti * 128:(ti + 1) * 128, :])
                dma(xx[ti % 3][:], xsrc[ti * 128:(ti + 1) * 128, :])
            GR = [(0, 384), (384, 384), (768, 256)]
            sv4 = sv[:].rearrange("p (h t) k -> p h t k", t=2)
            sif4 = sif[:].rearrange("p (h t) k -> p h t k", t=2)
            cand4 = cand[:].rearrange("p h (a b) -> p h a b", a=16)

            def A0(ti):
                M = mx[ti % 3]; S3 = s3b[ti % 2]; D3 = d3b[ti % 2]
                for g, (c0, w) in enumerate(GR):
                    act(junk[:, 0:w], M[:, c0:c0 + w], AF.Square, scale=float(w) ** -0.5, accum=S3[:, g:g + 1])
                act(D3[:, 0:3], S3[:, 0:3], AF.Sqrt, bias=epsT[:, 0:1], scale=1.0)

            def A1(ti):
                M = mx[ti % 3]; D3 = d3b[ti % 2]
                recip(r3[:, 0:3], D3[:, 0:3])
                for g, (c0, w) in enumerate(GR):
                    ts('pool', mtmp[:, c0:c0 + w], M[:, c0:c0 + w], r3[:, g:g + 1], 1.0, ALU.mult, ALU.mult)
                tt('pool', mixn[:], mtmp[:], gout[:], ALU.mult)
                for dc in range(8):
                    tr(tp[:, dc * 128:(dc + 1) * 128], mixn[:, dc * 128:(dc + 1) * 128], idb[:])
                cp('act', mixT[:], tp[:].rearrange("p (c t) -> p c t", c=8))
                for hf in range(2):
                    for dc in range(8):
                        mm(pA[:, hf * 512:(hf + 1) * 512], mixT[:, dc, :], Wo[:, dc, hf * 512:(hf + 1) * 512],
                           start=(dc == 0), stop=(dc == 7))

            def A2a(ti):
                X = xx[ti % 3]; XN = xnew[ti % 2]; r0 = ti * 128
                cp('act', wo_sb[:], pA[:])
                tt('pool', XN[:], X[:], wo_sb[:], ALU.add)
                dma(out[r0:r0 + 128, :], XN[:])
                act(junk2[:], XN[:], AF.Square, scale=1.0 / 32.0, accum=s4[:, 0:1])
                act(d4[:, 0:1], s4[:, 0:1], AF.Sqrt, bias=epsT[:, 0:1], scale=1.0)

            def A2b(ti):
                XN = xnew[ti % 2]; H2T = h2T[ti % 2]; r0 = ti * 128; SC = sc2[ti % 2]
                recip(r4[:, 0:1], d4[:, 0:1])
                ts('pool', mtmp[:], XN[:], r4[:, 0:1], 1.0, ALU.mult, ALU.mult)
                tt('pool', hn2[:], mtmp[:], gffn[:], ALU.mult)
                for dc in range(8):
                    tr(tp[:, dc * 128:(dc + 1) * 128], hn2[:, dc * 128:(dc + 1) * 128], idb[:])
                cp('act', H2T[:], tp[:].rearrange("p (c t) -> p c t", c=8))
                dma(HT[:, :, r0:r0 + 128].rearrange("c p t -> p c t"), H2T[:])
                for h in range(8):
                    for dc in range(8):
                        mm(pA[:, h * 128:(h + 1) * 128], Wqb[:, dc, h * 128:(h + 1) * 128], H2T[:, dc, :],
                           start=(dc == 0), stop=(dc == 7))
                cp('act', qT[:], pA[:].rearrange("p (h t) -> p h t", h=8))
                for h in range(8):
                    mm(pS[:, h * 256:(h + 1) * 256], qT[:, h, :], kbd[:, h, :])
                cp('act', SC[:], pS[:])

            def B1(ti):
                SC = sc2[ti % 2]
                segs = [(SC[:, g * 128:(g + 1) * 128], wk[:, g * 128:(g + 1) * 128]) for g in range(16)]
                for g in range(16):
                    vmax(sv[:, g, 0:8], segs[g][0])
                for g in range(16):
                    vmaxidx(si[:, g, 0:8], sv[:, g, 0:8], segs[g][0])
                    vmrep(segs[g][1], sv[:, g, 0:8], segs[g][0], NEG)
                for g in range(16):
                    vmax(sv[:, g, 8:16], segs[g][1])
                for g in range(16):
                    vmaxidx(si[:, g, 8:16], sv[:, g, 8:16], segs[g][1])
                cp('dve', sif[:], si[:])

            def B2a(ti):
                tt('dve', cand4, sv4[:, :, 0, :][:, :, :, None].to_broadcast([P, 8, 16, 16]),
                   sv4[:, :, 1, :][:, :, None, :].to_broadcast([P, 8, 16, 16]), ALU.add)
                for h in range(8):
                    vmax(best[:, h, 0:8], cand[:, h, :])
                for h in range(8):
                    vmaxidx(pos[:, h, 0:8], best[:, h, 0:8], cand[:, h, :])
                    vmrep(wk2[:, h, :], best[:, h, 0:8], cand[:, h, :], NEG)
                for h in range(8):
                    vmax(best[:, h, 8:16], wk2[:, h, :])
                for h in range(8):
                    vmaxidx(pos[:, h, 8:16], best[:, h, 8:16], wk2[:, h, :])
                tt('dve', dlt[:], best[:], best[:, :, 0:1].to_broadcast([P, 8, 16]), ALU.subtract)
                act(ex[:], dlt[:], AF.Exp)

            def B2b(ti):
                R = rt[ti % 2]; r0 = ti * 128
                tss(pa[:], pos[:], 4, ALU.logical_shift_right)
                tss(pb[:], pos[:], 15, ALU.bitwise_and)
                red(zz[:], ex[:])
                cp('dve', paf[:], pa[:]); cp('dve', pbf[:], pb[:])
                recip(rz[:], zz[:])
                iob = io16[:][:, None, None, :].to_broadcast([P, 8, 16, 16])
                eqs = (eq, eq2); prs = (pr, pr2); pfs = (paf, pbf)
                for which in range(2):
                    tt('dve', eqs[which][:], iob, pfs[which][:][:, :, :, None].to_broadcast([P, 8, 16, 16]), ALU.is_equal)
                G = IJG[:, 2, :].rearrange("p (h k) -> p h k", h=8)
                tt('dve', G, ex[:], rz[:][:, :, None].to_broadcast([P, 8, 16]), ALU.mult)
                for which in range(2):
                    tt('dve', prs[which][:], eqs[which][:], sif4[:, :, which, :][:, :, None, :].to_broadcast([P, 8, 16, 16]), ALU.mult)
                for which in range(2):
                    red(IJG[:, which, :].rearrange("p (h k) -> p h k", h=8), prs[which][:])
                for w3 in range(3):
                    tr(pR[:, w3 * 128:(w3 + 1) * 128], IJG[:, w3, :], idf[:])
                cp('act', R[:], pR[:, 0:384].rearrange("p (w t) -> p w t", w=3))
                dma(RT[:, :, r0:r0 + 128].rearrange("w p t -> p w t"), R[:])

            load(0); load(1); load(2)
            A0(0); A0(1)
            for dc in range(8):
                dma(wtmp[dc % 2][:], w_out[l, dc * 128:(dc + 1) * 128, :]); cp('act', Wo[:, dc, :], wtmp[dc % 2][:])
                dma(wtmp[2 + dc % 2][:], wq[l, dc * 128:(dc + 1) * 128, :]); cp('dve', Wqb[:, dc, :], wtmp[2 + dc % 2][:])
            dma(kbd[:], keysbd[l])
            dma(gout[:], out_norm[l:l + 1, :].to_broadcast([P, 1024]))
            dma(gffn[:], ffn_norm[l:l + 1, :].to_broadcast([P, 1024]))
            dma(idb[:], c_identb); dma(idf[:], c_identf); dma(io16[:], c_io16)
            A1(0); A2a(0); A2b(0)
            for ti in range(32):
                if ti + 3 < 32:
                    load(ti + 3)
                if ti + 1 < 32:
                    A1(ti + 1)
                B1(ti)
                if ti + 1 < 32:
                    A2a(ti + 1)
                if ti + 2 < 32:
                    A0(ti + 2)
                B2a(ti)
                if ti + 1 < 32:
                    A2b(ti + 1)
                B2b(ti)
            S.emit()

    def phase4(l):
        T = 256
        with ExitStack() as es:
            sb = lambda n, sh, dt: es.enter_context(nc.sbuf_tensor(nm(n), sh, dt))
            pst = lambda n, sh, dt: es.enter_context(nc.psum_tensor(nm(n), sh, dt))
            iota = sb("iota", [P, P], F32)
            Wbuf = [sb("Wbuf%d" % i, [P, T, 128], BF16) for i in range(2)]
            hT = [sb("hT%d" % i, [P, 8, T], BF16) for i in range(2)]
            r3 = [sb("r3_%d" % i, [P, 3, T], F32) for i in range(2)]
            NG = 8
            GI = [sb("GI%d" % i, [P, P], BF16) for i in range(NG)]
            OJ = [sb("OJ%d" % i, [P, P], BF16) for i in range(NG)]
            NBU = 8
            UV = [sb("UV%d" % i, [P, 2048], BF16) for i in range(NBU)]
            UTc = [UV[i][:, 0:1024] for i in range(NBU)]
            Vc = [UV[i][:, 1024:2048] for i in range(NBU)]
            NBV = NBU
            A = [sb("A%d" % i, [P, T], BF16) for i in range(3)]
            A2 = [sb("A2_%d" % i, [P, T], BF16) for i in range(3)]
            xx = [sb("xx%d" % i, [P, 1024], F32) for i in range(2)]
            xo = [sb("xo%d" % i, [P, 1024], F32) for i in range(2)]
            py = [pst("py%d" % i, [P, 512], F32) for i in range(4)]
            pSb = [pst("pS%d" % i, [P, 512], F32) for i in range(3)]
            pS = [pSb[i][:, 0:T] for i in range(3)]
            pW = [pst("pW%d" % i, [P, 512], F32) for i in range(1)]
            dma(iota[:], c_iota)
            ngroups = NT // T
            total = ngroups * 128

            def gload(gi):
                dma(hT[gi % 2][:], HT[:, :, gi * T:(gi + 1) * T].rearrange("c p t -> p c t"))
                dma(r3[gi % 2][:], RT[:, :, gi * T:(gi + 1) * T].rearrange("w p t -> p w t"))

            def uload(n):
                if n < total:
                    dma(UV[n % NBU][:], UVb[l, n % 128])

            def vload(n):
                pass

            wcs = [0]
            pend = []

            def wgen_dve(g, t):
                R = r3[g % 2]
                gi_t = GI[wcs[0] % NG]; oj_t = OJ[wcs[0] % NG]; wcs[0] += 1
                ts('dve', gi_t[:], iota[:], R[:, 0, t:t + 1], R[:, 2, t:t + 1], ALU.is_equal, ALU.mult)
                ts('dve', oj_t[:], iota[:], R[:, 1, t:t + 1], None, ALU.is_equal)
                pend.append((g, t, gi_t, oj_t))

            def wgen_pe():
                while pend:
                    g, t, gi_t, oj_t = pend.pop(0)
                    pw = pW[0]
                    mm(pw[:, (t % 4) * 128:(t % 4 + 1) * 128], oj_t[:], gi_t[:])
                    if t % 4 == 3:
                        cp('act', Wbuf[g % 2][:, t - 3:t + 1, :], pw[:].rearrange("p (t i) -> p t i", t=4))

            def s_mm(n):
                g = n // 128
                Uc = UTc[n % NBU]; H = hT[g % 2]
                for dc in range(8):
                    mm(pS[n % 3], Uc[:, dc * 128:(dc + 1) * 128], H[:, dc, :], start=(dc == 0), stop=(dc == 7))

            gload(0)
            for n in range(NBU - 1):
                uload(n)
            for n in range(NBV - 1):
                vload(n)
            for t in range(T):
                wgen_dve(0, t)
                if t % 2 == 1:
                    wgen_pe()
            s_mm(0); s_mm(1)
            for gi in range(ngroups):
                if gi + 1 < ngroups:
                    gload(gi + 1)
                for tt_ in range(2):
                    r0 = gi * T + tt_ * 128
                    dma(xx[tt_][:], out[r0:r0 + 128, :])
                for c in range(128):
                    n = gi * 128 + c
                    uload(n + NBU - 1)
                    vload(n + NBV - 1)
                    if n + 2 < total:
                        s_mm(n + 2)
                    Vv = Vc[n % NBV]
                    a = A[n % 3]; a2 = A2[n % 3]
                    act(a[:], pS[n % 3], AF.Gelu)
                    tt('dve', a2[:], a[:], Wbuf[gi % 2][:, :, c], ALU.mult)
                    wgen_pe()
                    if gi + 1 < ngroups:
                        wgen_dve(gi + 1, 2 * c); wgen_dve(gi + 1, 2 * c + 1)
                    for tt_ in range(2):
                        for hh in range(2):
                            mm(py[tt_ * 2 + hh][:], a2[:, tt_ * 128:(tt_ + 1) * 128], Vv[:, hh * 512:(hh + 1) * 512],
                               start=(c == 0), stop=(c == 127))
                wgen_pe()
                for tt_ in range(2):
                    r0 = gi * T + tt_ * 128
                    for hh in range(2):
                        tt('dve', xo[tt_][:, hh * 512:(hh + 1) * 512], xx[tt_][:, hh * 512:(hh + 1) * 512], py[tt_ * 2 + hh][:], ALU.add)
                    dma(out[r0:r0 + 128, :], xo[tt_][:])
            S.emit()

    stages = []
    for l in range(2):
        xs = x if l == 0 else out
        stages.append(('p1_%d' % l, lambda l=l, xs=xs: phase1(l, xs)))
        stages.append(('p2_%d' % l, lambda l=l: phase2(l)))
        stages.append(('p3_%d' % l, lambda l=l, xs=xs: phase3(l, xs)))
        stages.append(('p4_%d' % l, lambda l=l: phase4(l)))
    for name, fn in stages:
        fn()
        if stop_after == name:
            break
    return nc


def _consts():
    f32 = np.float32
    bf = ml_dtypes.bfloat16
    c = {}
    c["c_identb"] = np.eye(P, dtype=f32).astype(bf)
    c["c_identf"] = np.eye(P, dtype=f32)
    pos = np.arange(2048, dtype=f32)

    def invf(dim):
        return (f32(10000.0) ** (-(np.arange(0, dim, 2, dtype=f32) / f32(dim)))).astype(f32)
    r = np.arange(P)
    d = r % 64
    f = invf(64)[d % 32]
    ang = (pos[None, :] * f[:, None]).astype(f32)
    sgn = np.where(d < 32, -1.0, 1.0).astype(f32)
    c["c_ct1"] = np.cos(ang).astype(f32)
    c["c_st1"] = (np.sin(ang) * sgn[:, None]).astype(f32)
    e = d % 32
    which = d // 32
    fa = invf(32)[e % 16]
    rowp = np.floor(pos / 64).astype(f32); colp = (pos % 64).astype(f32)
    pv_ = np.where(which[:, None] == 0, rowp[None, :], colp[None, :]).astype(f32)
    anga = (pv_ * fa[:, None]).astype(f32)
    sgna = np.where(e < 16, -1.0, 1.0).astype(f32)
    c["c_cta"] = np.cos(anga).astype(f32)
    c["c_sta"] = (np.sin(anga) * sgna[:, None]).astype(f32)
    p1 = np.where(d < 32, r + 32, r - 32)
    pa = np.where(e < 16, r + 16, r - 16)
    m1 = np.zeros((P, P), f32); m1[p1, r] = 1.0
    ma = np.zeros((P, P), f32); ma[pa, r] = 1.0
    c["c_psw1"] = m1.astype(bf); c["c_pswa"] = ma.astype(bf)
    c["c_bones"] = ((r[:, None] // 64) == (r[None, :] // 64)).astype(f32).astype(bf)
    cc = np.arange(3968)
    dd = cc[None, :] - 1920 - r[:, None]
    ad = np.abs(dd)
    fb = (ad <= 64).astype(f32) + ((dd % 4 == 0) & (ad <= 256)).astype(f32) + ((dd % 16 == 0) & (ad <= 1024)).astype(f32)
    c["c_tb"] = np.where(fb > 0, 8.0 * np.log(np.maximum(fb, 1.0)), -30000.0).astype(f32).astype(bf)
    c["c_tc"] = np.where(ad <= 128, 0.0, -30000.0).astype(f32).astype(bf)
    c["c_iota"] = np.tile(np.arange(P, dtype=f32)[None, :], (P, 1))
    c["c_io16"] = np.tile(np.arange(16, dtype=f32)[None, :], (P, 1))
    return c


def _shared_inputs(attn_norm, w_in, qk_gain, sink_logits, out_norm, w_out, ffn_norm, peer_wq, peer_keys, peer_u, peer_v):
    f32 = np.float32
    m = dict(attn_norm=np.ascontiguousarray(attn_norm, f32), out_norm=np.ascontiguousarray(out_norm, f32),
             ffn_norm=np.ascontiguousarray(ffn_norm, f32), w_in=np.ascontiguousarray(w_in, f32),
             w_out=np.ascontiguousarray(w_out, f32), wq=np.ascontiguousarray(peer_wq, f32),
             pu=np.ascontiguousarray(peer_u, f32), pv=np.ascontiguousarray(peer_v, f32))
    sel = [(0, 0)] * 3 + [(0, 1)] + [(1, 0)] * 3 + [(1, 1)] * 3 + [(2, 0)] * 2 + [(2, 1)]
    gc = np.zeros((2, P, 13), f32)
    for l in range(2):
        for ci, (mi, qi) in enumerate(sel):
            gc[l, :, ci] = np.tile(np.asarray(qk_gain[l, mi, qi], f32), 2)
    m["gaincol"] = gc
    m["sink"] = np.ascontiguousarray(np.asarray(sink_logits, f32).reshape(1, 8))
    kb = np.zeros((2, P, 8, 256), f32)
    pk = np.asarray(peer_keys, f32)
    for p in range(2):
        kb[:, p * 64:(p + 1) * 64, :, p * 128:(p + 1) * 128] = pk[:, :, p].transpose(0, 3, 1, 2)
    m["keysbd"] = kb
    m.update(_consts())
    return m


def kernel(x, attn_norm, w_in, qk_gain, sink_logits, out_norm, w_out, ffn_norm, peer_wq, peer_keys, peer_u, peer_v):
    x = np.asarray(x, np.float32)
    shared = _shared_inputs(attn_norm, w_in, qk_gain, sink_logits, out_norm, w_out, ffn_norm,
                            peer_wq, peer_keys, peer_u, peer_v)
    nc = build()
    in_maps = []
    for c in range(8):
        m = dict(shared)
        m["x"] = np.ascontiguousarray(x[2 * c:2 * c + 2].reshape(NT, 1024))
        in_maps.append(m)
    res = run_bass_kernel_spmd(nc, in_maps, core_ids=list(range(8)))
    outs = [np.asarray(r["out"], np.float32).reshape(2, 2048, 1024) for r in res.results]
    return np.concatenate(outs, axis=0)
```
